# Optimizing a Trainium2 kernel written in Bass

```python
import jax, jax.numpy as jnp
from jax import lax
import numpy as np

D_MODEL = 1024
BATCH = 8
SEQ = 4096
DEPTH = 1

SWA_HEADS = 8
SWA_HEAD_DIM = 64
DILATED_PATTERNS = ((128, 1), (512, 4), (2048, 16))
MLA_HEADS = 8
MLA_NOPE_DIM = 64
MLA_ROPE_DIM = 32
MLA_V_DIM = 64
MLA_Q_RANK = 256
MLA_KV_RANK = 256
ROPE_THETA = 10000.0
Q_BLOCK = 128
SWA_WIDTH = SWA_HEADS * SWA_HEAD_DIM
MLA_WIDTH = MLA_HEADS * MLA_V_DIM
MIX_WIDTH = SWA_WIDTH + MLA_WIDTH
IN_SPLITS = (SWA_WIDTH, 2 * SWA_WIDTH, 3 * SWA_WIDTH, 3 * SWA_WIDTH + MLA_Q_RANK,
             3 * SWA_WIDTH + MLA_Q_RANK + MLA_KV_RANK)
IN_PROJ_WIDTH = 3 * SWA_WIDTH + MLA_Q_RANK + MLA_KV_RANK + MLA_ROPE_DIM
PEER_HEADS = 8
PEER_N_KEYS = 128
PEER_N_EXPERTS = PEER_N_KEYS * PEER_N_KEYS
PEER_TOPK = 16
PEER_QUERY_DIM = 256
PEER_HALF_DIM = PEER_QUERY_DIM // 2
PEER_TOKEN_CHUNK = 128
NORM_EPS = 1e-6
NEG_INF = -1e30

kernel_name = 'hybrid_dilated_mla_peer_block'


def rms_norm(x, g):
    xf = x.astype(jnp.float32)
    y = xf * lax.rsqrt(jnp.mean(xf * xf, axis=-1, keepdims=True) + NORM_EPS)
    return (y * g.astype(jnp.float32)).astype(x.dtype)


def rope_tables(seq, dim):
    inv = 1.0 / (ROPE_THETA ** (jnp.arange(0, dim, 2, dtype=jnp.float32) / dim))
    ang = jnp.arange(seq, dtype=jnp.float32)[:, None] * inv[None, :]
    return jnp.cos(ang), jnp.sin(ang)


def apply_rope(x, cos, sin):
    half = x.shape[-1] // 2
    x1 = x[..., :half].astype(jnp.float32)
    x2 = x[..., half:].astype(jnp.float32)
    return jnp.concatenate([x1 * cos - x2 * sin, x2 * cos + x1 * sin], axis=-1).astype(x.dtype)


def dilated_branch(q, k, v, window, dilation):
    b, h, s, hd = q.shape
    span = window // dilation
    unit = span * dilation
    s_pad = -(-s // unit) * unit
    m = s_pad // dilation
    nb = m // span
    pad = ((0, 0), (0, 0), (0, s_pad - s), (0, 0))

    def to_blocks(t):
        t = jnp.pad(t, pad).reshape(b, h, m, dilation, hd).transpose(0, 1, 3, 2, 4)
        return t.reshape(b, h, dilation, nb, span, hd)

    def with_prev(t):
        prev = jnp.pad(t[:, :, :, :-1], ((0, 0), (0, 0), (0, 0), (1, 0), (0, 0), (0, 0)))
        return jnp.concatenate([prev, t], axis=4)

    qb = to_blocks(q)
    kk = with_prev(to_blocks(k))
    vv = with_prev(to_blocks(v))
    i = jnp.arange(span)[:, None]
    c = jnp.arange(2 * span)[None, :]
    rel = span + i - c
    band = (rel >= 0) & (rel <= span)
    blk = jnp.arange(nb)[:, None, None]
    mask = band[None] & ((blk > 0) | (c >= span)[None])
    sc = jnp.einsum('bhrnqd,bhrnkd->bhrnqk', qb, kk, preferred_element_type=jnp.float32) * (hd ** -0.5)
    sc = jnp.where(mask, sc, NEG_INF)
    lse = jax.nn.logsumexp(sc, axis=-1)
    p = jnp.exp(sc - lse[..., None]).astype(v.dtype)
    o = jnp.einsum('bhrnqk,bhrnkd->bhrnqd', p, vv)
    o = o.reshape(b, h, dilation, m, hd).transpose(0, 1, 3, 2, 4).reshape(b, h, s_pad, hd)[:, :, :s]
    lse = lse.reshape(b, h, dilation, m).transpose(0, 1, 3, 2).reshape(b, h, s_pad)[:, :, :s]
    return o, lse


def dilated_attention(q, k, v):
    outs, lses = [], []
    for window, dilation in DILATED_PATTERNS:
        o, l = dilated_branch(q, k, v, window, dilation)
        outs.append(o)
        lses.append(l)
    wts = jax.nn.softmax(jnp.stack(lses, axis=0), axis=0)
    out = jnp.sum(wts[..., None] * jnp.stack(outs, axis=0).astype(jnp.float32), axis=0)
    return out.astype(q.dtype)


def mla_attention(c_q, c_kv, k_rope, q_norm_g, kv_norm_g, w_uq, w_uk, w_uv, cos, sin):
    b, s, _ = c_q.shape
    q = jnp.einsum('bsr,rf->bsf', rms_norm(c_q, q_norm_g), w_uq)
    q = q.reshape(b, s, MLA_HEADS, MLA_NOPE_DIM + MLA_ROPE_DIM).transpose(0, 2, 1, 3)
    q_nope = q[..., :MLA_NOPE_DIM]
    q_rope = apply_rope(q[..., MLA_NOPE_DIM:], cos, sin)
    ckv = rms_norm(c_kv, kv_norm_g)
    k_nope = jnp.einsum('bsr,rf->bsf', ckv, w_uk).reshape(b, s, MLA_HEADS, MLA_NOPE_DIM).transpose(0, 2, 1, 3)
    v = jnp.einsum('bsr,rf->bsf', ckv, w_uv).reshape(b, s, MLA_HEADS, MLA_V_DIM).transpose(0, 2, 1, 3)
    k_r = apply_rope(k_rope, cos, sin)
    scale = (MLA_NOPE_DIM + MLA_ROPE_DIM) ** -0.5
    nb = s // Q_BLOCK
    qn_b = q_nope.reshape(b, MLA_HEADS, nb, Q_BLOCK, MLA_NOPE_DIM).transpose(2, 0, 1, 3, 4)
    qr_b = q_rope.reshape(b, MLA_HEADS, nb, Q_BLOCK, MLA_ROPE_DIM).transpose(2, 0, 1, 3, 4)
    kpos = jnp.arange(s)

    def block(args):
        qn, qr, start = args
        sc = (jnp.einsum('bhqd,bhkd->bhqk', qn, k_nope, preferred_element_type=jnp.float32)
              + jnp.einsum('bhqd,bkd->bhqk', qr, k_r, preferred_element_type=jnp.float32)) * scale
        qpos = start + jnp.arange(Q_BLOCK)
        sc = jnp.where(kpos[None, :] <= qpos[:, None], sc, NEG_INF)
        p = jax.nn.softmax(sc, axis=-1).astype(v.dtype)
        return jnp.einsum('bhqk,bhkd->bhqd', p, v)

    o = lax.map(block, (qn_b, qr_b, jnp.arange(nb) * Q_BLOCK))
    return o.transpose(1, 0, 3, 2, 4).reshape(b, s, MLA_HEADS * MLA_V_DIM)


def peer_ffn(x, w_query, sub_keys, expert_u, expert_v):
    b, s, d = x.shape
    n_tok = b * s
    n_chunks = n_tok // PEER_TOKEN_CHUNK

    def chunk(tc):
        q = (tc @ w_query).reshape(PEER_TOKEN_CHUNK, PEER_HEADS, 2, PEER_HALF_DIM)
        sc = jnp.einsum('thpc,hpnc->thpn', q, sub_keys, preferred_element_type=jnp.float32)
        s_top, i_top = lax.top_k(sc, PEER_TOPK)
        cand = s_top[:, :, 0, :, None] + s_top[:, :, 1, None, :]
        cand_idx = i_top[:, :, 0, :, None] * PEER_N_KEYS + i_top[:, :, 1, None, :]
        cand = cand.reshape(PEER_TOKEN_CHUNK, PEER_HEADS, PEER_TOPK * PEER_TOPK)
        cand_idx = cand_idx.reshape(PEER_TOKEN_CHUNK, PEER_HEADS, PEER_TOPK * PEER_TOPK)
        best, pos = lax.top_k(cand, PEER_TOPK)
        idx = jnp.take_along_axis(cand_idx, pos, axis=-1)
        gate = jax.nn.softmax(best, axis=-1)
        u = expert_u[idx]
        hid = jax.nn.gelu(jnp.einsum('cd,chkd->chk', tc, u, preferred_element_type=jnp.float32),
                          approximate=False) * gate
        vv = expert_v[idx]
        return jnp.einsum('chk,chkd->cd', hid.astype(vv.dtype), vv)

    out = lax.map(chunk, x.reshape(n_chunks, PEER_TOKEN_CHUNK, d))
    return out.reshape(b, s, d)


def setup_inputs(seed: int = 0) -> dict:
    key = jax.random.key(seed)
    ks = jax.random.split(key, 16)
    L = DEPTH

    def w(k, shape, fan_in):
        return jax.random.normal(k, shape, jnp.float32) * (fan_in ** -0.5)

    def gain(k, shape):
        return 1.0 + 0.01 * jax.random.normal(k, shape, jnp.float32)

    return {
        'x': jax.random.normal(ks[0], (BATCH, SEQ, D_MODEL), jnp.float32),
        'attn_norm_g': gain(ks[1], (L, D_MODEL)),
        'w_in': w(ks[2], (L, D_MODEL, IN_PROJ_WIDTH), D_MODEL),
        'mla_q_norm_g': gain(ks[3], (L, MLA_Q_RANK)),
        'mla_kv_norm_g': gain(ks[4], (L, MLA_KV_RANK)),
        'w_uq': w(ks[5], (L, MLA_Q_RANK, MLA_HEADS * (MLA_NOPE_DIM + MLA_ROPE_DIM)), MLA_Q_RANK),
        'w_uk': w(ks[6], (L, MLA_KV_RANK, MLA_HEADS * MLA_NOPE_DIM), MLA_KV_RANK),
        'w_uv': w(ks[7], (L, MLA_KV_RANK, MLA_HEADS * MLA_V_DIM), MLA_KV_RANK),
        'w_o': w(ks[8], (L, MIX_WIDTH, D_MODEL), MIX_WIDTH),
        'ffn_norm_g': gain(ks[9], (L, D_MODEL)),
        'peer_w_query': w(ks[10], (L, D_MODEL, PEER_HEADS * PEER_QUERY_DIM), D_MODEL),
        'peer_sub_keys': w(ks[11], (L, PEER_HEADS, 2, PEER_N_KEYS, PEER_HALF_DIM), PEER_HALF_DIM),
        'peer_u': w(ks[12], (L, PEER_N_EXPERTS, D_MODEL), D_MODEL),
        'peer_v': w(ks[13], (L, PEER_N_EXPERTS, D_MODEL), PEER_TOPK),
        'final_norm_g': gain(ks[14], (D_MODEL,)),
    }


def reference(x, attn_norm_g, w_in, mla_q_norm_g, mla_kv_norm_g, w_uq, w_uk, w_uv, w_o,
              ffn_norm_g, peer_w_query, peer_sub_keys, peer_u, peer_v, final_norm_g):
    b, s, _ = x.shape
    cos_a, sin_a = rope_tables(s, SWA_HEAD_DIM)
    cos_b, sin_b = rope_tables(s, MLA_ROPE_DIM)
    for layer in range(DEPTH):
        h = rms_norm(x, attn_norm_g[layer])
        proj = jnp.einsum('bsd,df->bsf', h, w_in[layer])
        q_a, k_a, v_a, c_q, c_kv, k_rope = jnp.split(proj, IN_SPLITS, axis=-1)

        def heads(t):
            return t.reshape(b, s, SWA_HEADS, SWA_HEAD_DIM).transpose(0, 2, 1, 3)

        q_a = apply_rope(heads(q_a), cos_a, sin_a)
        k_a = apply_rope(heads(k_a), cos_a, sin_a)
        o_a = dilated_attention(q_a, k_a, heads(v_a)).transpose(0, 2, 1, 3).reshape(b, s, SWA_WIDTH)
        o_b = mla_attention(c_q, c_kv, k_rope, mla_q_norm_g[layer], mla_kv_norm_g[layer],
                            w_uq[layer], w_uk[layer], w_uv[layer], cos_b, sin_b)
        mixed = jnp.concatenate([o_a, o_b], axis=-1)
        x = x + jnp.einsum('bsf,fd->bsd', mixed, w_o[layer])
        h = rms_norm(x, ffn_norm_g[layer])
        x = x + peer_ffn(h, peer_w_query[layer], peer_sub_keys[layer], peer_u[layer], peer_v[layer])
    return rms_norm(x, final_norm_g)
```

```python
import numpy as np
import ml_dtypes
from contextlib import ExitStack
import concourse.bass as bass
import concourse.mybir as mybir
from concourse.bass_utils import run_bass_kernel_spmd

F32 = mybir.dt.float32
BF16 = mybir.dt.bfloat16
U32 = mybir.dt.uint32
ALU = mybir.AluOpType
AF = mybir.ActivationFunctionType
AX = mybir.AxisListType

ENGS = ["pe", "act", "dve", "pool", "sp"]
SAME_ENGINE_SYNC = {"pool", "act", "dve"}
SAME_ENGINE_DIST = 8

SEQ = 4096
DM = 1024
NT = SEQ // 128
EPS = 1e-6
THETA = 10000.0
NEXP_BLK = 128


class Sched:
    def __init__(self, nc, stack):
        self.nc = nc
        self.stack = stack
        self.q = {e: [] for e in ENGS}
        self.cnt = {e: 0 for e in ENGS}
        self.known = {e: {} for e in ENGS}
        self.lastw = {}
        self.readers = {}
        self.esem = {e: stack.enter_context(nc.semaphore("es_" + e)) for e in ENGS}
        self.dsem = {}
        self.dcnt = {}
        self.n_ins = 0
        self.n_wait = 0

    def _emit_waits(self, eng, deps):
        need = {}
        for key, val in deps:
            if key == eng and (eng not in SAME_ENGINE_SYNC or self.cnt[eng] - val >= SAME_ENGINE_DIST):
                continue
            if self.known[eng].get(key, 0) >= val:
                continue
            if need.get(key, 0) < val:
                need[key] = val
        for key, val in need.items():
            self.known[eng][key] = val
            sem = self.esem[key] if key in self.esem else self.dsem[key]
            self.q[eng].append(("w", sem, val))
            self.n_wait += 1

    def _deps(self, reads, writes):
        deps = []
        for b in reads:
            deps.extend(self.lastw.get(b, {}).items())
        for b in writes:
            deps.extend(self.lastw.get(b, {}).items())
            deps.extend(self.readers.get(b, {}).items())
        return deps

    def _record(self, key, val, reads, writes):
        for b in reads:
            r = self.readers.setdefault(b, {})
            if r.get(key, 0) < val:
                r[key] = val
        for b in writes:
            if self.readers.get(b):
                self.lastw[b] = {key: val}
                self.readers[b] = {}
            else:
                w = self.lastw.setdefault(b, {})
                if w.get(key, 0) < val:
                    w[key] = val

    def op(self, eng, fn, reads=(), writes=()):
        self._emit_waits(eng, self._deps(reads, writes))
        self.cnt[eng] += 1
        self.q[eng].append(("i", fn))
        self.n_ins += 1
        self._record(eng, self.cnt[eng], reads, writes)

    def dma(self, stream, out, in_, reads=(), writes=(), queue="sp", **kw):
        if stream not in self.dsem:
            self.dsem[stream] = self.stack.enter_context(self.nc.semaphore("ds_" + stream))
            self.dcnt[stream] = 0
        self._emit_waits(queue, self._deps(reads, writes))
        self.dcnt[stream] += 16
        self.q[queue].append(("d", out, in_, self.dsem[stream], kw))
        self.n_ins += 1
        self._record(stream, self.dcnt[stream], reads, writes)

    def dma_group(self, stream, items, queue="sp"):
        if stream not in self.dsem:
            self.dsem[stream] = self.stack.enter_context(self.nc.semaphore("ds_" + stream))
            self.dcnt[stream] = 0
        deps = []
        for (o, i, R, W, kw) in items:
            deps.extend(self._deps(R, W))
        self._emit_waits(queue, deps)
        for (o, i, R, W, kw) in items:
            self.dcnt[stream] += 16
            self.q[queue].append(("d", o, i, self.dsem[stream], kw))
            self.n_ins += 1
        for (o, i, R, W, kw) in items:
            self._record(stream, self.dcnt[stream], R, W)

    def wait_all(self, eng):
        deps = [(e, self.cnt[e]) for e in ENGS if self.cnt[e] > 0 and e != eng]
        deps += [(s, v) for s, v in self.dcnt.items() if v > 0]
        self._emit_waits(eng, deps)

    def barrier(self):
        for e in ENGS:
            self.wait_all(e)

    def replay(self, block):
        names = {"pe": "tensor", "act": "scalar", "dve": "vector", "pool": "gpsimd", "sp": "sync"}

        def make(e):
            def body(E):
                sem = self.esem[e]
                for item in self.q[e]:
                    if item[0] == "w":
                        E.wait_ge(item[1], item[2])
                    elif item[0] == "i":
                        item[1](E).then_inc(sem, 1)
                    else:
                        _, out, in_, dsem, kw = item
                        E.dma_start(out=out, in_=in_, **kw).then_inc(dsem, 16)
            return body

        for e in ENGS:
            getattr(block, names[e])(make(e))


class Rot:
    def __init__(self, items):
        self.items = list(items)
        self.i = 0

    def next(self):
        v = self.items[self.i % len(self.items)]
        self.i += 1
        return v


def _host_consts():
    t = np.arange(SEQ, dtype=np.float32)
    invA = (1.0 / (THETA ** (np.arange(0, 64, 2, dtype=np.float32) / 64.0))).astype(np.float32)
    invM = (1.0 / (THETA ** (np.arange(0, 32, 2, dtype=np.float32) / 32.0))).astype(np.float32)
    angA = (t[None, :] * invA[:, None]).astype(np.float32)
    angM = (t[None, :] * invM[:, None]).astype(np.float32)
    tabs = np.zeros((4, 128, SEQ), np.float32)
    for p in range(128):
        f = p % 32
        tabs[0, p] = np.cos(angA[f])
        tabs[1, p] = np.sin(angA[f]) * (1.0 if (p % 64) < 32 else -1.0)
    for q in range(32):
        f = q % 16
        tabs[2, 64 + q] = np.cos(angM[f])
        tabs[3, 64 + q] = np.sin(angM[f]) * (-1.0 if q < 16 else 1.0)
    k = np.arange(128)[:, None]
    qq = np.arange(128)[None, :]
    mask2 = np.concatenate([(k >= qq), (k <= qq)], axis=1).astype(np.float32)
    return {
        "c_tabs": tabs.astype(ml_dtypes.bfloat16),
        "c_ident": np.eye(128, dtype=np.float32).astype(ml_dtypes.bfloat16),
        "c_identf": np.eye(128, dtype=np.float32),
        "c_mask2": mask2.astype(ml_dtypes.bfloat16),
        "c_iota": np.tile(np.arange(128, dtype=np.float32)[None, :], (128, 1)).astype(ml_dtypes.bfloat16),
        "c_iota16": np.tile(np.arange(16, dtype=np.float32)[None, :], (128, 1)),
    }


def build_program(debug=None):
    nc = bass.Bass("TRN2", target_bir_lowering=False)

    def din(name, shape, dt=F32):
        return nc.dram_tensor(name, list(shape), dt, kind="ExternalInput").ap()

    def dscr(name, shape, dt):
        kind = "ExternalOutput" if (debug and name in debug) else "Internal"
        return nc.dram_tensor(name, list(shape), dt, kind=kind).ap()

    x = din("x", [SEQ, DM])
    g_attn = din("attn_norm_g", [DM])
    w_in = din("w_in", [DM, 2080])
    g_q = din("mla_q_norm_g", [256])
    g_kv = din("mla_kv_norm_g", [256])
    w_uq = din("w_uq", [256, 768])
    w_uk = din("w_uk", [256, 512])
    w_uv = din("w_uv", [256, 512])
    w_o = din("w_o", [DM, DM])
    g_ffn = din("ffn_norm_g", [DM])
    w_query = din("peer_w_query", [DM, 2048])
    sub_keys = din("peer_sub_keys", [16 * 128, 128])
    peer_u = din("peer_u", [16384, DM])
    peer_v = din("peer_v", [16384, DM])
    g_fin = din("final_norm_g", [DM])
    c_tabs = din("c_tabs", [4, 128, SEQ], BF16)
    c_ident = din("c_ident", [128, 128], BF16)
    c_identf = din("c_identf", [128, 128], F32)
    c_mask2 = din("c_mask2", [128, 256], BF16)
    c_iota = din("c_iota", [128, 128], BF16)
    c_iota16 = din("c_iota16", [128, 16], F32)
    out = nc.dram_tensor("out", [SEQ, DM], F32, kind="ExternalOutput").ap()

    qkT = dscr("qkT", [8, 128, SEQ], BF16)
    vA = dscr("vA", [SEQ, 768], BF16)
    mq = dscr("mq", [8, 96, SEQ], BF16)
    mk = dscr("mk", [8, 96, SEQ], BF16)
    vM = dscr("vM", [SEQ, 768], BF16)
    mixT = dscr("mixT", [8, 128, SEQ], BF16)
    x1d = dscr("x1d", [SEQ, DM], F32)
    h2Td = dscr("h2Td", [128, 8, SEQ], BF16)
    Gd = dscr("Gd", [128, 128, SEQ], BF16)
    UTd = dscr("UTd", [128, 128, 8 * 128], BF16)
    Vbd = dscr("Vbd", [16384, DM], BF16)

    stop_after = (debug or {}).get("_stop", "E")

    with ExitStack() as top:
        S = Sched(nc, top)
        ps = [top.enter_context(nc.psum_tensor(f"ps{k}", [128, 512], F32)) for k in range(8)]

        def psf(k):
            return ps[k][:]

        def psb(k):
            return ps[k][:].bitcast(BF16)

        def mm(o, l, r, st, sp_, R, W):
            S.op("pe", lambda E, o=o, l=l, r=r, st=st, sp_=sp_: E.matmul(o, lhsT=l, rhs=r, start=st, stop=sp_), R, W)

        def tr(o, i, idn, R, W):
            S.op("pe", lambda E, o=o, i=i, idn=idn: E.transpose(o, i, idn), R, W)

        def tt(eng, o, a, b, op, R, W):
            S.op(eng, lambda E, o=o, a=a, b=b, op=op: E.tensor_tensor(out=o, in0=a, in1=b, op=op), R, W)

        def ts(eng, o, a, s1, s2, op0, op1, R, W):
            if op1 is None:
                S.op(eng, lambda E, o=o, a=a, s1=s1, op0=op0: E.tensor_scalar(out=o, in0=a, scalar1=s1, scalar2=None, op0=op0), R, W)
            else:
                S.op(eng, lambda E, o=o, a=a, s1=s1, s2=s2, op0=op0, op1=op1: E.tensor_scalar(out=o, in0=a, scalar1=s1, scalar2=s2, op0=op0, op1=op1), R, W)

        def stt(eng, o, a, sc, b, op0, op1, R, W):
            S.op(eng, lambda E, o=o, a=a, sc=sc, b=b, op0=op0, op1=op1: E.scalar_tensor_tensor(out=o, in0=a, scalar=sc, in1=b, op0=op0, op1=op1), R, W)

        def cp(eng, o, i, R, W):
            if eng == "act":
                S.op(eng, lambda E, o=o, i=i: E.copy(out=o, in_=i), R, W)
            else:
                S.op(eng, lambda E, o=o, i=i: E.tensor_copy(out=o, in_=i), R, W)

        def act(o, i, func, R, W, scale=1.0, bias=None, accum=None):
            kw = {}
            if bias is not None:
                kw["bias"] = bias
            if accum is not None:
                kw["accum_out"] = accum
            S.op("act", lambda E, o=o, i=i, func=func, scale=scale, kw=kw: E.activation(out=o, in_=i, func=func, scale=scale, **kw), R, W)

        def recip(o, i, R, W):
            S.op("dve", lambda E, o=o, i=i: E.reciprocal(out=o, in_=i), R, W)

        def recipf(o, i, R, W):
            S.op("dve", lambda E, o=o, i=i: E.reciprocal_approx_fast(out=o, in_=i), R, W)

        def memset(eng, o, v, W):
            S.op(eng, lambda E, o=o, v=v: E.memset(o, v), (), W)

        castrot = Rot(["act", "dve", "pool"])

        def phase_U(ph):
            sb = lambda n, s, d: ph.enter_context(nc.sbuf_tensor(n, s, d))
            ident = sb("U_ident", [128, 128], BF16)
            S.dma("c0", ident[:], c_ident, writes=["ident"])
            NS = 3
            ust = [sb(f"U_ust{k}", [128, DM], F32) for k in range(NS)]
            vst = [sb(f"U_vst{k}", [128, DM], F32) for k in range(NS)]
            ubf = [sb(f"U_ubf{k}", [128, DM], BF16) for k in range(NS)]
            vbf = [sb(f"U_vbf{k}", [128, DM], BF16) for k in range(NS)]
            utl = [sb(f"U_utl{k}", [128, 8 * 128], BF16) for k in range(NS)]
            for i in range(128):
                s = i % NS
                pb = i % 2
                S.dma_group(f"Ul{s}", [
                    (ust[s][:], peer_u[i * 128:(i + 1) * 128, :], [], [f"ust{s}"], {}),
                    (vst[s][:], peer_v[i * 128:(i + 1) * 128, :], [], [f"vst{s}"], {}),
                ])
                cp(castrot.next(), ubf[s][:], ust[s][:], [f"ust{s}"], [f"ubf{s}"])
                cp(castrot.next(), vbf[s][:], vst[s][:], [f"vst{s}"], [f"vbf{s}"])
                for c in range(8):
                    tr(psb(pb)[:, c * 128:(c + 1) * 128], ubf[s][:, c * 128:(c + 1) * 128], ident[:], [f"ubf{s}", "ident"], [f"ps{pb}"])
                cp("dve" if i % 2 == 0 else "act", utl[s][:], psb(pb), [f"ps{pb}"], [f"utl{s}"])
                S.dma_group(f"Us{s}", [
                    (UTd[i], utl[s][:], [f"utl{s}"], ["UTd"], {}),
                    (Vbd[i * 128:(i + 1) * 128, :], vbf[s][:], [f"vbf{s}"], ["Vbd"], {}),
                ])

        def phase_P(ph):
            sb = lambda n, s, d: ph.enter_context(nc.sbuf_tensor(n, s, d))
            ident = sb("P_ident", [128, 128], BF16)
            ones = sb("P_ones", [128, 128], BF16)
            memset("dve", ones[:], 1.0, ["ones"])
            win = sb("P_win", [128, 8, 2080], BF16)
            winsw = sb("P_winsw", [128, 8, 96], BF16)
            wuq = sb("P_wuq", [128, 2, 768], BF16)
            wuqsw = sb("P_wuqsw", [128, 2, 768], BF16)
            wuk = sb("P_wuk", [128, 2, 512], BF16)
            wuv = sb("P_wuv", [128, 2, 512], BF16)
            wst = [sb(f"P_wst{k}", [128, 2080], F32) for k in range(4)]
            g1 = sb("P_g1", [128, 8], F32)
            gq = sb("P_gq", [128, 2], F32)
            gkv = sb("P_gkv", [128, 2], F32)
            nck = {"allow_slow_non_contiguous": True}
            S.dma_group("c0", [
                (ident[:], c_ident, [], ["ident"], {}),
                (g1[:], g_attn.rearrange("(c p) -> p c", p=128), [], ["g1"], nck),
                (gq[:], g_q.rearrange("(c p) -> p c", p=128), [], ["gq"], nck),
                (gkv[:], g_kv.rearrange("(c p) -> p c", p=128), [], ["gkv"], nck),
            ])
            for c in range(8):
                s = c % 4
                S.dma(f"Pw{s}", wst[s][:], w_in[c * 128:(c + 1) * 128, :], writes=[f"wst{s}"])
                cp(castrot.next(), win[:, c, :], wst[s][:], [f"wst{s}"], [f"win{c}"])
            WIN = [f"win{c}" for c in range(8)]
            cp("pool", winsw[:, :, 0:64], win[:, :, 1984:2048], WIN, ["winsw"])
            cp("pool", winsw[:, :, 64:80], win[:, :, 2064:2080], WIN, ["winsw"])
            cp("pool", winsw[:, :, 80:96], win[:, :, 2048:2064], WIN, ["winsw"])
            k = 0
            for (wd, wt, ncol, wn) in ((w_uq, wuq, 768, "wuq"), (w_uk, wuk, 512, "wuk"), (w_uv, wuv, 512, "wuv")):
                for c in range(2):
                    s = k % 4
                    k += 1
                    S.dma(f"Pw{s}", wst[s][:, 0:ncol], wd[c * 128:(c + 1) * 128, :], writes=[f"wst{s}"])
                    cp(castrot.next(), wt[:, c, :], wst[s][:, 0:ncol], [f"wst{s}"], [wn])
            wq4 = wuq[:].rearrange("p c (h f) -> p c h f", f=96)
            wqs4 = wuqsw[:].rearrange("p c (h f) -> p c h f", f=96)
            for c in range(2):
                cp("pool", wqs4[:, c, :, 0:64], wq4[:, c, :, 0:64], ["wuq"], ["wuqsw"])
                cp("pool", wqs4[:, c, :, 64:80], wq4[:, c, :, 80:96], ["wuq"], ["wuqsw"])
                cp("pool", wqs4[:, c, :, 80:96], wq4[:, c, :, 64:80], ["wuq"], ["wuqsw"])

            xin = [sb(f"P_xin{k}", [128, DM], F32) for k in range(3)]
            junk = sb("P_junk", [128, DM], BF16)
            xn = [sb(f"P_xn{k}", [128, DM], BF16) for k in range(2)]
            st1 = [sb(f"P_st{k}", [128, 4], F32) for k in range(3)]
            hT = [sb(f"P_hT{k}", [128, 8, 512], BF16) for k in range(2)]
            tabs = [sb(f"P_tabs{k}", [128, 4, 512], BF16) for k in range(2)]
            o32 = [sb(f"P_o32{k}", [128, 512], F32) for k in range(2)]
            pcp = [sb(f"P_pcp{k}", [128, 512], F32) for k in range(2)]
            tmp = [sb(f"P_tmp{k}", [128, 512], F32) for k in range(2)]
            qko = [sb(f"P_qko{k}", [128, 512], BF16) for k in range(3)]
            vst = [sb(f"P_vst{k}", [128, 768], BF16) for k in range(2)]
            vmst = [sb(f"P_vmst{k}", [128, 768], BF16) for k in range(2)]
            for k in range(2):
                memset("pool", vst[k][:], 1.0, [f"vst{k}"])
                memset("pool", vmst[k][:], 1.0, [f"vmst{k}"])
            sq = [sb(f"P_sq{k}", [128, 512], BF16) for k in range(2)]
            rstd = sb("P_rstd", [128, 512], F32)
            cqn = sb("P_cqn", [128, 2, 512], BF16)
            ckvn = sb("P_ckvn", [128, 2, 512], BF16)
            mqo = [sb(f"P_mqo{k}", [96, 512], BF16) for k in range(2)]
            mks = sb("P_mks", [96, 8, 512], BF16)
            kr = sb("P_kr", [96, 512], BF16)
            t1 = [sb(f"P_t1{k}", [96, 512], F32) for k in range(2)]
            t2 = [sb(f"P_t2{k}", [96, 512], F32) for k in range(2)]

            prot = Rot([2, 3, 4])
            nslot = Rot([0, 1])
            qslot = Rot([0, 1, 2])

            def vview(t):
                return t[:].rearrange("p (m x) -> p m x", x=192).rearrange("p m (a b) -> p m a b", b=64)[:, :, 0:3:2, :]

            def p_tiles(ck):
                hs = ck % 2
                tok0 = ck * 512
                S.dma(f"Ptab{hs}", tabs[hs][:], c_tabs[:, :, tok0:tok0 + 512].rearrange("f p t -> p f t"), writes=[f"tabs{hs}"])
                for tl in range(4):
                    ti = ck * 4 + tl
                    xs = ti % 3
                    ns = ti % 2
                    pb = ti % 2
                    S.dma(f"Px{xs}", xin[xs][:], x[ti * 128:(ti + 1) * 128, :], writes=[f"xin{xs}"])
                    S.op("act", lambda E, xs=xs: E.memzero(st1[xs][:, 0:1]), [], [f"ss{xs}"])
                    act(junk[:], xin[xs][:], AF.Square, [f"xin{xs}"], [f"junk{xs}", f"ss{xs}"], accum=st1[xs][:, 0:1])
                    act(st1[xs][:, 1:2], st1[xs][:, 0:1], AF.Sqrt, [f"ss{xs}"], [f"sd{xs}"], scale=1.0 / DM, bias=EPS)
                    recip(st1[xs][:, 2:3], st1[xs][:, 1:2], [f"sd{xs}"], [f"rs{xs}"])
                    act(xn[ns][:], xin[xs][:], AF.Copy, [f"xin{xs}", f"rs{xs}"], [f"xn{ns}"], scale=st1[xs][:, 2:3])
                    for c in range(8):
                        tr(psb(pb)[:, c * 128:(c + 1) * 128], xn[ns][:, c * 128:(c + 1) * 128], ident[:], [f"xn{ns}", "ident"], [f"ps{pb}"])
                    tt("dve", hT[hs][:, :, tl * 128:(tl + 1) * 128], psb(pb).rearrange("p (c t) -> p c t", t=128),
                       g1[:].unsqueeze(2).to_broadcast([128, 8, 128]), ALU.mult, [f"ps{pb}", "g1"], [f"hT{hs}_{tl}"])

            p_tiles(0)
            for ck in range(8):
                hs = ck % 2
                tok0 = ck * 512
                HK = [f"hT{hs}_{tl}" for tl in range(4)]
                tk = f"tabs{hs}"
                psec = (debug or {}).get("_psec", "qvlkmw")
                def emit_lat(li):
                    (lcol, gl, gname, dst, dkey) = ((1536, gq, "gq", cqn, "cqn"), (1792, gkv, "gkv", ckvn, "ckvn"))[li]
                    for blk in range(2):
                        b = 5 + blk
                        for c in range(8):
                            mm(psf(b), win[:, c, lcol + blk * 128:lcol + (blk + 1) * 128], hT[hs][:, c, :], c == 0, c == 7, [f"win{c}"] + HK, [f"ps{b}"])
                        act(sq[blk][:], psf(b), AF.Square, [f"ps{b}"], [f"sq{blk}"])
                    for blk in range(2):
                        mm(psf(7), ones[:], sq[blk][:], blk == 0, blk == 1, ["ones", f"sq{blk}"], ["ps7"])
                    act(rstd[:], psf(7), AF.Sqrt, ["ps7"], ["rstd"], scale=1.0 / 256.0, bias=EPS)
                    recip(rstd[:], rstd[:], ["rstd"], ["rstd"])
                    for blk in range(2):
                        stt("dve", dst[:, blk, :], psf(5 + blk), gl[:, blk:blk + 1], rstd[:], ALU.mult, ALU.mult,
                            [f"ps{5 + blk}", "rstd", gname], [f"{dkey}{blk}"])
                def emit_fb(fb):
                    col0 = fb * 128
                    b = prot.next()
                    for c in range(8):
                        mm(psf(b), win[:, c, col0:col0 + 128], hT[hs][:, c, :], c == 0, c == 7, [f"win{c}"] + HK, [f"ps{b}"])
                    s2 = nslot.next()
                    qs = qslot.next()
                    tt("dve", o32[s2][:], psf(b), tabs[hs][:, 0, :], ALU.mult, [f"ps{b}", tk], [f"o32{s2}"])
                    tt("dve", tmp[s2][0:32, :], psf(b)[32:64, :], tabs[hs][32:64, 1, :], ALU.mult, [f"ps{b}", tk], [f"tmp{s2}a"])
                    tt("dve", tmp[s2][32:64, :], psf(b)[0:32, :], tabs[hs][0:32, 1, :], ALU.mult, [f"ps{b}", tk], [f"tmp{s2}b"])
                    tt("dve", tmp[s2][64:96, :], psf(b)[96:128, :], tabs[hs][96:128, 1, :], ALU.mult, [f"ps{b}", tk], [f"tmp{s2}c"])
                    tt("dve", tmp[s2][96:128, :], psf(b)[64:96, :], tabs[hs][64:96, 1, :], ALU.mult, [f"ps{b}", tk], [f"tmp{s2}d"])
                    tt("pool", qko[qs][:], o32[s2][:], tmp[s2][:], ALU.add, [f"o32{s2}"] + [f"tmp{s2}{z}" for z in "abcd"], [f"qko{qs}"])
                    S.dma(f"Pqk{qs}", qkT[fb, :, tok0:tok0 + 512], qko[qs][:], reads=[f"qko{qs}"], writes=["qkT"])

                def emit_va(tl):
                    ti = ck * 4 + tl
                    b = prot.next()
                    vs = ti % 2
                    for c in range(8):
                        mm(psf(b), hT[hs][:, c, tl * 128:(tl + 1) * 128], win[:, c, 1024:1536], c == 0, c == 7, [f"win{c}", f"hT{hs}_{tl}"], [f"ps{b}"])
                    cp("act", vview(vst[vs]), psf(b).rearrange("p (m a b) -> p m a b", a=2, b=64), [f"ps{b}"], [f"vst{vs}"])
                    S.dma(f"Pva{vs}", vA[ti * 128:(ti + 1) * 128, :], vst[vs][:], reads=[f"vst{vs}"], writes=["vA"])

                for fb in range(8):
                    if "q" in psec:
                        emit_fb(fb)
                    if fb % 2 == 1 and "v" in psec:
                        emit_va(fb // 2)
                    if fb == 2 and "l" in psec:
                        emit_lat(0)
                    if fb == 5 and "l" in psec:
                        emit_lat(1)
                if ck + 1 < 8:
                    p_tiles(ck + 1)
                CQ = ["cqn0", "cqn1"]
                CKV = ["ckvn0", "ckvn1"]
                if "k" not in psec:
                    continue
                bA = prot.next()
                for c in range(8):
                    mm(psf(bA)[0:96, :], win[:, c, 1984:2080], hT[hs][:, c, :], c == 0, c == 7, [f"win{c}"] + HK, [f"ps{bA}"])
                bB = prot.next()
                for c in range(8):
                    mm(psf(bB)[0:96, :], winsw[:, c, :], hT[hs][:, c, :], c == 0, c == 7, ["winsw"] + HK, [f"ps{bB}"])
                tt("dve", t1[0][64:96, :], psf(bA)[64:96, :], tabs[hs][64:96, 2, :], ALU.mult, [f"ps{bA}", tk], ["t10"])
                tt("dve", t2[0][64:96, :], psf(bB)[64:96, :], tabs[hs][64:96, 3, :], ALU.mult, [f"ps{bB}", tk], ["t20"])
                tt("dve", kr[64:96, :], t1[0][64:96, :], t2[0][64:96, :], ALU.add, ["t10", "t20"], ["kr"])
                cp("dve", mks[64:96, :, :], kr[64:96, :].unsqueeze(1).to_broadcast([32, 8, 512]), ["kr"], ["mksr"])
                if "m" not in psec:
                    continue
                for h in range(8):
                    bA = prot.next()
                    for c in range(2):
                        mm(psf(bA)[0:96, :], wuq[:, c, h * 96:(h + 1) * 96], cqn[:, c, :], c == 0, c == 1, ["wuq"] + CQ, [f"ps{bA}"])
                    bB = prot.next()
                    for c in range(2):
                        mm(psf(bB)[0:96, :], wuqsw[:, c, h * 96:(h + 1) * 96], cqn[:, c, :], c == 0, c == 1, ["wuqsw"] + CQ, [f"ps{bB}"])
                    ms = h % 2
                    cp("act", mqo[ms][0:64, :], psf(bA)[0:64, :], [f"ps{bA}"], [f"mqo{ms}n"])
                    tt("dve", t1[ms][64:96, :], psf(bA)[64:96, :], tabs[hs][64:96, 2, :], ALU.mult, [f"ps{bA}", tk], [f"t1{ms}"])
                    tt("dve", t2[ms][64:96, :], psf(bB)[64:96, :], tabs[hs][64:96, 3, :], ALU.mult, [f"ps{bB}", tk], [f"t2{ms}"])
                    tt("dve", mqo[ms][64:96, :], t1[ms][64:96, :], t2[ms][64:96, :], ALU.add, [f"t1{ms}", f"t2{ms}"], [f"mqo{ms}r"])
                    S.dma(f"Pmq{ms}", mq[h, :, tok0:tok0 + 512], mqo[ms][:], reads=[f"mqo{ms}n", f"mqo{ms}r"], writes=["mq"])
                    bK = prot.next()
                    for c in range(2):
                        mm(psf(bK)[0:64, :], wuk[:, c, h * 64:(h + 1) * 64], ckvn[:, c, :], c == 0, c == 1, ["wuk"] + CKV, [f"ps{bK}"])
                    cp("act", mks[0:64, h, :], psf(bK)[0:64, :], [f"ps{bK}"], [f"mks{h}"])
                S.dma("Pmk", mk[:, :, tok0:tok0 + 512].rearrange("h p t -> p h t"), mks[:], reads=["mksr"] + [f"mks{h}" for h in range(8)], writes=["mk"])
                for tl in (range(4) if "w" in psec else []):
                    ti = ck * 4 + tl
                    b = prot.next()
                    vs = ti % 2
                    for c in range(2):
                        mm(psf(b), ckvn[:, c, tl * 128:(tl + 1) * 128], wuv[:, c, :], c == 0, c == 1, ["wuv"] + CKV, [f"ps{b}"])
                    cp("act", vview(vmst[vs]), psf(b).rearrange("p (m a b) -> p m a b", a=2, b=64), [f"ps{b}"], [f"vmst{vs}"])
                    S.dma(f"Pvm{vs}", vM[ti * 128:(ti + 1) * 128, :], vmst[vs][:], reads=[f"vmst{vs}"], writes=["vM"])

        def u_alloc(ph):
            sb = lambda n, s, d: ph.enter_context(nc.sbuf_tensor(n, s, d))
            NS = 3
            ub = {
                "ident": sb("U_ident", [128, 128], BF16),
                "ust": [sb(f"U_ust{k}", [128, DM], F32) for k in range(NS)],
                "vst": [sb(f"U_vst{k}", [128, DM], F32) for k in range(NS)],
                "ubf": [sb(f"U_ubf{k}", [128, DM], BF16) for k in range(NS)],
                "vbf": [sb(f"U_vbf{k}", [128, DM], BF16) for k in range(NS)],
                "utl": [sb(f"U_utl{k}", [128, 8 * 128], BF16) for k in range(NS)],
                "NS": NS,
            }
            S.dma("Uc", ub["ident"][:], c_ident, writes=["Uident"])
            return ub

        def u_gen(ub, bank=7):
            NS = ub["NS"]
            crotA = Rot(["act", "act", "act", "dve"])
            crotM = Rot(["dve", "pool"])

            class _C:
                def next(self_):
                    return (crotM if ub.get("inM") else crotA).next()
            crot = _C()

            def load(i):
                s = i % NS
                S.dma_group(f"Ul{s}", [
                    (ub["ust"][s][:], peer_u[i * 128:(i + 1) * 128, :], [], [f"Uust{s}"], {}),
                    (ub["vst"][s][:], peer_v[i * 128:(i + 1) * 128, :], [], [f"Uvst{s}"], {}),
                ], queue="act")
            load(0)
            load(1)
            for i in range(128):
                s = i % NS
                if i + 2 < 128:
                    load(i + 2)
                cp(crot.next(), ub["ubf"][s][:], ub["ust"][s][:], [f"Uust{s}"], [f"Uubf{s}"])
                cp(crot.next(), ub["vbf"][s][:], ub["vst"][s][:], [f"Uvst{s}"], [f"Uvbf{s}"])
                for c in range(8):
                    tr(psb(bank)[:, c * 128:(c + 1) * 128], ub["ubf"][s][:, c * 128:(c + 1) * 128], ub["ident"][:], [f"Uubf{s}", "Uident"], [f"ps{bank}"])
                cp("dve", ub["utl"][s][:], psb(bank), [f"ps{bank}"], [f"Uutl{s}"])
                S.dma_group(f"Us{s}", [
                    (UTd[i], ub["utl"][s][:], [f"Uutl{s}"], ["UTd"], {}),
                    (Vbd[i * 128:(i + 1) * 128, :], ub["vbf"][s][:], [f"Uvbf{s}"], ["Vbd"], {}),
                ])
                yield

        def phase_A(ph, ugen=None):
            sb = lambda n, s, d: ph.enter_context(nc.sbuf_tensor(n, s, d))
            mask2 = sb("A_mask2", [128, 256], BF16)
            S.dma("c0", mask2[:], c_mask2, writes=["mask2"])
            QT = [sb(f"A_QT{k}", [128, SEQ], BF16) for k in range(2)]
            KT = [sb(f"A_KT{k}", [128, SEQ], BF16) for k in range(2)]
            Vp = [sb(f"A_Vp{k}", [128, 32, 192], BF16) for k in range(3)]
            QTp = [sb(f"A_QTp{k}", [128, SEQ], BF16) for k in range(2)]
            KTp = [sb(f"A_KTp{k}", [128, SEQ], BF16) for k in range(2)]
            acc = [sb(f"A_acc{k}", [128, SEQ], F32) for k in range(2)]
            NPT = 8
            PT = [sb(f"A_PT{k}", [128, 256], BF16) for k in range(NPT)]
            rden = [sb(f"A_rden{k}", [128, 1024], F32) for k in range(2)]
            mixs = [sb(f"A_mixs{k}", [128, SEQ], BF16) for k in range(2)]
            srot = Rot([0, 1, 2, 3])
            prot = Rot([4, 5, 6])
            ptrot = Rot(list(range(NPT)))
            mrot = Rot(["pool", "dve"])
            D = 5
            PATS = (1, 4, 16)

            def load_qk(m):
                s = m % 2
                S.dma_group(f"Aq{s}", [
                    (QT[s][:, hf * 2048:(hf + 1) * 2048], qkT[m, :, hf * 2048:(hf + 1) * 2048], ["qkT"], [f"QT{s}"], {}) for hf in range(2)
                ] + [
                    (KT[s][:, hf * 2048:(hf + 1) * 2048], qkT[4 + m, :, hf * 2048:(hf + 1) * 2048], ["qkT"], [f"KT{s}"], {}) for hf in range(2)
                ])

            def load_vp(m, pi):
                d = PATS[pi]
                nb = 32 // d
                vs = pi
                vsrc = vA.rearrange("(n i r) c -> i r n c", i=128, r=d)
                items = []
                for r in range(d):
                    step = 8 if nb > 8 else nb
                    for n0 in range(0, nb, step):
                        items.append((Vp[vs][:, r * nb + n0:r * nb + n0 + step, :], vsrc[:, r, n0:n0 + step, 192 * m:192 * m + 192],
                                      ["vA"], [f"Vp{vs}"], {}))
                S.dma_group(f"Av{vs}", items)

            def prep_perm(m, pi):
                if pi == 0:
                    return
                d = PATS[pi]
                s = m % 2
                slot = pi - 1
                for hf in range(2):
                    r0, r1 = hf * d // 2, (hf + 1) * d // 2
                    srcq = QT[s][:, :].rearrange("p (m r) -> p r m", r=d)[:, r0:r1, :]
                    dstq = QTp[slot][:, :].rearrange("p (r m) -> p r m", r=d)[:, r0:r1, :]
                    srck = KT[s][:, :].rearrange("p (m r) -> p r m", r=d)[:, r0:r1, :]
                    dstk = KTp[slot][:, :].rearrange("p (r m) -> p r m", r=d)[:, r0:r1, :]
                    cp("act", dstq, srcq, [f"QT{s}"], [f"QTp{slot}_{hf}"])
                    cp("dve", dstk, srck, [f"KT{s}"], [f"KTp{slot}_{hf}"])

            its = []
            for m in range(4):
                for hh in range(2):
                    for pi, d in enumerate(PATS):
                        nb = 32 // d
                        pidx = 0
                        for r in range(d):
                            for n in range(nb):
                                pidx += 1
                                its.append(dict(m=m, pi=pi, d=d, nb=nb, hh=hh, r=r, n=n, k=pi,
                                                pat_first=(pidx == D + 1),
                                                head_last=(pi == 2 and r == d - 1 and n == nb - 1)))
            load_qk(0)
            for pi_ in range(3):
                load_vp(0, pi_)
            state = {"pvb": None}

            def stage1(it):
                m, d, r, n, hh, k = it["m"], it["d"], it["r"], it["n"], it["hh"], it["k"]
                s = m % 2
                if it["pat_first"]:
                    pi_ = it["pi"]
                    if hh == 0 and pi_ == 0:
                        prep_perm(m, 1)
                        prep_perm(m, 2)
                        if m > 0:
                            load_vp(m, 2)
                    if hh == 1 and pi_ == 0 and m + 1 < 4:
                        load_qk(m + 1)
                    if hh == 1 and pi_ >= 1 and m + 1 < 4:
                        load_vp(m + 1, pi_ - 1)
                rows = slice(hh * 64, (hh + 1) * 64)
                nb_ = it["nb"]
                if d == 1:
                    qsrc, ksrc = QT[s], KT[s]
                    rk = [f"KT{s}", f"QT{s}"]
                else:
                    slot = it["pi"] - 1
                    qsrc, ksrc = QTp[slot], KTp[slot]
                    rk = [f"KTp{slot}_0", f"KTp{slot}_1", f"QTp{slot}_0", f"QTp{slot}_1"]
                blk = r * nb_ + n
                qap = qsrc[rows, blk * 128:(blk + 1) * 128]
                sbk = srot.next()
                sps = psf(sbk)[:, 0:256]
                skey = f"ps{sbk}"
                pt = ptrot.next()
                it["pt"] = pt
                c0 = 0 if n > 0 else 128
                if n > 0:
                    mm(sps[:, 0:128], ksrc[rows, (blk - 1) * 128:blk * 128], qap, True, True, rk, [skey])
                mm(sps[:, 128:256], ksrc[rows, blk * 128:(blk + 1) * 128], qap, True, True, rk, [skey])
                act(PT[pt][:, c0:256], sps[:, c0:256], AF.Exp, [skey], [f"PT{pt}"], scale=0.125)
                tt("pool", PT[pt][:, c0:256], PT[pt][:, c0:256], mask2[:, c0:256], ALU.mult, [f"PT{pt}", "mask2"], [f"PT{pt}"])

            def stage2(it):
                m, d, r, n, hh, k, pi, nb = it["m"], it["d"], it["r"], it["n"], it["hh"], it["k"], it["pi"], it["nb"]
                s = m % 2
                vs = pi
                pt = it["pt"]
                grp = 4 if d < 16 else 2
                j = n % grp
                if j == 0:
                    state["pvb"] = prot.next()
                pvb = state["pvb"]
                vcols = slice(0, 128) if hh == 0 else slice(64, 192)
                pvo = psf(pvb)[:, j * 128:(j + 1) * 128]
                if n > 0:
                    mm(pvo, Vp[vs][:, r * nb + n - 1, vcols], PT[pt][:, 0:128], True, False, [f"Vp{vs}", f"PT{pt}"], [f"ps{pvb}"])
                mm(pvo, Vp[vs][:, r * nb + n, vcols], PT[pt][:, 128:256], n == 0, True, [f"Vp{vs}", f"PT{pt}"], [f"ps{pvb}"])
                if j == grp - 1:
                    n_first = n - (grp - 1)
                    t0 = n_first * 128 * d + r
                    span = grp * 128 * d
                    if d == 1:
                        ak = [f"acc{hh}_{t0 // 512}"]
                    elif d == 4:
                        ak = [f"acc{hh}_{(t0 // 512) + z}" for z in range(4)]
                    else:
                        ak = [f"acc{hh}_{z}" for z in range(8)]
                    dst = acc[hh][:, t0:t0 + span - d + 1:d]
                    src = psf(pvb)[:, 0:grp * 128]
                    if pi == 0:
                        cp("dve", dst, src, [f"ps{pvb}"], ak)
                    else:
                        tt("dve", dst, dst, src, ALU.add, [f"ps{pvb}"] + ak, ak)
                if it["head_last"]:
                    h2_ = hh
                    for cc in range(4):
                        cs = slice(cc * 1024, (cc + 1) * 1024)
                        ak = [f"acc{h2_}_{2 * cc}", f"acc{h2_}_{2 * cc + 1}"]
                        rs = cc % 2
                        if h2_ == 0:
                            recip(rden[rs][0:64, :], acc[0][64:128, cs], ak, [f"rden{rs}"])
                            tt("pool", mixs[s][0:64, cs], acc[0][0:64, cs], rden[rs][0:64, :], ALU.mult, ak + [f"rden{rs}"], [f"mixs{s}_{h2_}{cc}"])
                        else:
                            recip(rden[rs][64:128, :], acc[1][0:64, cs], ak, [f"rden{rs}"])
                            tt("dve", mixs[s][64:128, cs], acc[1][64:128, cs], rden[rs][64:128, :], ALU.mult, ak + [f"rden{rs}"], [f"mixs{s}_{h2_}{cc}"])
                    if hh == 1:
                        S.dma(f"Amx{s}", mixT[m], mixs[s][:], reads=[f"mixs{s}_{a_}{cc}" for a_ in range(2) for cc in range(4)], writes=["mixT"])

            NI = len(its)
            for idx in range(NI + D):
                if idx < NI:
                    stage1(its[idx])
                if idx >= D:
                    stage2(its[idx - D])
                if ugen is not None and idx % 12 == 11:
                    next(ugen, None)

        def phase_M(ph, ugen=None):
            sb = lambda n, s, d: ph.enter_context(nc.sbuf_tensor(n, s, d))
            if ugen is not None:
                UB["inM"] = True
            mask2 = sb("M_mask2", [128, 256], BF16)
            S.dma("c0", mask2[:], c_mask2, writes=["mask2"])
            QM = [sb(f"M_QM{k}", [96, SEQ], BF16) for k in range(2)]
            KM = [sb(f"M_KM{k}", [96, SEQ], BF16) for k in range(2)]
            VM = [sb(f"M_VM{k}", [128, 32, 128], BF16) for k in range(2)]
            NPT = 8
            PT = [sb(f"M_PT{k}", [128, 512], BF16) for k in range(NPT)]
            rden = [sb(f"M_rden{k}", [128, 512], F32) for k in range(2)]
            mixs = [sb(f"M_mixs{k}", [128, SEQ], BF16) for k in range(2)]
            srot = Rot([0, 1, 2, 3])
            arot = Rot([4, 5, 6])
            ptrot = Rot(list(range(NPT)))
            scale = 96.0 ** -0.5
            vsrc = vM.rearrange("(n i) c -> i n c", i=128)
            D = 5

            def load_head(h):
                s = h % 2
                m = h // 2
                c0 = 192 * m if h % 2 == 0 else 192 * m + 64
                items = []
                for hf in range(2):
                    items.append((QM[s][:, hf * 2048:(hf + 1) * 2048], mq[h, :, hf * 2048:(hf + 1) * 2048], ["mq"], [f"QM{s}"], {}))
                    items.append((KM[s][:, hf * 2048:(hf + 1) * 2048], mk[h, :, hf * 2048:(hf + 1) * 2048], ["mk"], [f"KM{s}"], {}))
                for n0 in range(0, 32, 8):
                    items.append((VM[s][:, n0:n0 + 8, :], vsrc[:, n0:n0 + 8, c0:c0 + 128], ["vM"], [f"VM{s}"], {}))
                S.dma_group(f"Ml{s}", items)

            its = []
            for h in range(8):
                hidx = 0
                for g in range(8):
                    for j in range(4 * g + 4):
                        hidx += 1
                        its.append(dict(h=h, g=g, j=j, head_first=(hidx == D + 1)))
            load_head(0)
            state = {"ab": None}

            def stage1(it):
                h, g, j = it["h"], it["g"], it["j"]
                s = h % 2
                if it["head_first"] and h + 1 < 8:
                    load_head(h + 1)
                b0 = max(0, j - 4 * g)
                cols = slice(b0 * 128, 512)
                sbk = srot.next()
                pt = ptrot.next()
                it["pt"] = pt
                mm(psf(sbk)[:, cols], KM[s][:, j * 128:(j + 1) * 128], QM[s][:, g * 512 + b0 * 128:(g + 1) * 512], True, True,
                   [f"KM{s}", f"QM{s}"], [f"ps{sbk}"])
                act(PT[pt][:, cols], psf(sbk)[:, cols], AF.Exp, [f"ps{sbk}"], [f"PT{pt}"], scale=scale)
                if j >= 4 * g:
                    dc = slice(b0 * 128, (b0 + 1) * 128)
                    tt("pool", PT[pt][:, dc], PT[pt][:, dc], mask2[:, 128:256], ALU.mult, [f"PT{pt}", "mask2"], [f"PT{pt}"])

            def stage2(it):
                h, g, j = it["h"], it["g"], it["j"]
                s = h % 2
                m = h // 2
                ms = m % 2
                pt = it["pt"]
                if j == 0:
                    state["ab"] = arot.next()
                ab = state["ab"]
                b0 = max(0, j - 4 * g)
                cols = slice(b0 * 128, 512)
                mm(psf(ab)[:, cols], VM[s][:, j, :], PT[pt][:, cols], j == 0, j == 4 * g + 3, [f"VM{s}", f"PT{pt}"], [f"ps{ab}"])
                if j == 4 * g + 3:
                    rs = g % 2
                    gs = slice(g * 512, (g + 1) * 512)
                    mk_ = f"mixs{ms}_{h % 2}{g}"
                    if h % 2 == 0:
                        recip(rden[rs][0:64, :], psf(ab)[64:128, :], [f"ps{ab}"], [f"rden{rs}"])
                        tt("dve", mixs[ms][0:64, gs], psf(ab)[0:64, :], rden[rs][0:64, :], ALU.mult, [f"ps{ab}", f"rden{rs}"], [mk_])
                    else:
                        recip(rden[rs][64:128, :], psf(ab)[0:64, :], [f"ps{ab}"], [f"rden{rs}"])
                        tt("dve", mixs[ms][64:128, gs], psf(ab)[64:128, :], rden[rs][64:128, :], ALU.mult, [f"ps{ab}", f"rden{rs}"], [mk_])
                    if g == 7 and h % 2 == 1:
                        S.dma(f"Mmx{ms}", mixT[4 + m], mixs[ms][:], reads=[f"mixs{ms}_{a}{gg}" for a in range(2) for gg in range(8)], writes=["mixT"])

            NI = len(its)
            for idx in range(NI + D):
                if idx < NI:
                    stage1(its[idx])
                if idx >= D:
                    stage2(its[idx - D])
                if ugen is not None and idx % 18 == 17:
                    next(ugen, None)
            if ugen is not None:
                for _ in ugen:
                    pass

        def phase_X(ph):
            sb = lambda n, s, d: ph.enter_context(nc.sbuf_tensor(n, s, d))
            ident = sb("X_ident", [128, 128], BF16)
            wo = sb("X_wo", [128, 8, DM], BF16)
            wst = [sb(f"X_wst{k}", [128, DM], F32) for k in range(4)]
            g2 = sb("X_g2", [128, 8], F32)
            S.dma_group("c0", [
                (ident[:], c_ident, [], ["ident"], {}),
                (g2[:], g_ffn.rearrange("(c p) -> p c", p=128), [], ["g2"], {"allow_slow_non_contiguous": True}),
            ])
            for c in range(8):
                s = c % 4
                S.dma(f"Xw{s}", wst[s][:], w_o[c * 128:(c + 1) * 128, :], writes=[f"wst{s}"])
                cp(castrot.next(), wo[:, c, :], wst[s][:], [f"wst{s}"], [f"wo{c}"])
            mixin = [sb(f"X_mix{k}", [128, 8, 512], BF16) for k in range(2)]
            xin = [sb(f"X_xin{k}", [128, DM], F32) for k in range(3)]
            x1 = [sb(f"X_x1{k}", [128, DM], F32) for k in range(3)]
            junk = sb("X_junk", [128, DM], BF16)
            xn = [sb(f"X_xn{k}", [128, DM], BF16) for k in range(2)]
            st1 = [sb(f"X_st{k}", [128, 4], F32) for k in range(3)]
            h2 = [sb(f"X_h2{k}", [128, 8, 128], BF16) for k in range(2)]
            prot = Rot([(2, 3), (4, 5), (6, 7)])
            def x_partA(ti):
                    ck, tl = divmod(ti, 4)
                    mslot = ck % 2
                    if tl == 0:
                        S.dma(f"Xm{mslot}", mixin[mslot][:], mixT[:, :, ck * 512:(ck + 1) * 512].rearrange("c p t -> p c t"), reads=["mixT"], writes=[f"mixin{mslot}"])
                    xs = ti % 3
                    ns = ti % 2
                    S.dma(f"Xx{xs}", xin[xs][:], x[ti * 128:(ti + 1) * 128, :], writes=[f"xin{xs}"])
                    ba, bb = prot.next()
                    for hf, b in ((0, ba), (1, bb)):
                        for c in range(8):
                            mm(psf(b), mixin[mslot][:, c, tl * 128:(tl + 1) * 128], wo[:, c, hf * 512:(hf + 1) * 512], c == 0, c == 7,
                               [f"mixin{mslot}", f"wo{c}"], [f"ps{b}"])
                        tt("dve", x1[xs][:, hf * 512:(hf + 1) * 512], psf(b), xin[xs][:, hf * 512:(hf + 1) * 512], ALU.add,
                           [f"ps{b}", f"xin{xs}"], [f"x1{xs}_{hf}"])
                    XK = [f"x1{xs}_0", f"x1{xs}_1"]
                    S.dma(f"Xo{xs}", x1d[ti * 128:(ti + 1) * 128, :], x1[xs][:], reads=XK, writes=["x1d"])
                    S.op("act", lambda E, xs=xs: E.memzero(st1[xs][:, 0:1]), [], [f"ss{xs}"])
                    act(junk[:], x1[xs][:], AF.Square, XK, [f"junk{xs}", f"ss{xs}"], accum=st1[xs][:, 0:1])
                    act(st1[xs][:, 1:2], st1[xs][:, 0:1], AF.Sqrt, [f"ss{xs}"], [f"sd{xs}"], scale=1.0 / DM, bias=EPS)
                    recip(st1[xs][:, 2:3], st1[xs][:, 1:2], [f"sd{xs}"], [f"rs{xs}"])
                    act(xn[ns][:], x1[xs][:], AF.Copy, XK + [f"rs{xs}"], [f"xn{ns}"], scale=st1[xs][:, 2:3])

            def x_partB(ti):
                    ns = ti % 2
                    pb = ti % 2
                    for c in range(8):
                        tr(psb(pb)[:, c * 128:(c + 1) * 128], xn[ns][:, c * 128:(c + 1) * 128], ident[:], [f"xn{ns}", "ident"], [f"ps{pb}"])
                    tt("dve", h2[ns][:], psb(pb).rearrange("p (c t) -> p c t", t=128),
                       g2[:].unsqueeze(2).to_broadcast([128, 8, 128]), ALU.mult, [f"ps{pb}", "g2"], [f"h2{ns}"])
                    S.dma(f"Xh{ns}", h2Td[:, :, ti * 128:(ti + 1) * 128], h2[ns][:], reads=[f"h2{ns}"], writes=["h2Td"])

            x_partA(0)
            for ti in range(NT):
                if ti + 1 < NT:
                    x_partA(ti + 1)
                x_partB(ti)

        def phase_O(ph):
            sb = lambda n, s, d: ph.enter_context(nc.sbuf_tensor(n, s, d))
            ident = sb("O_ident", [128, 128], BF16)
            identf = sb("O_identf", [128, 128], F32)
            iota = sb("O_iota", [128, 128], BF16)
            iota16 = sb("O_iota16", [128, 16], F32)
            S.dma_group("c0", [
                (ident[:], c_ident, [], ["ident"], {}),
                (identf[:], c_identf, [], ["identf"], {}),
                (iota[:], c_iota, [], ["iota"], {}),
                (iota16[:], c_iota16, [], ["iota16"], {}),
            ])
            wq = sb("O_wq", [128, 8, 2048], BF16)
            wst = [sb(f"O_wst{k}", [128, 1024], F32) for k in range(2)]
            eq = [sb(f"O_eq{k}", [128, 8, 16, 16], F32) for k in range(2)]
            wv = [wst[0][:], wst[1][:]]
            for k_ in range(2):
                fl = eq[k_][:].rearrange("p a b c -> p (a b c)")
                wv += [fl[:, 0:1024], fl[:, 1024:2048]]
            NWS = len(wv)
            wi = 0
            for c in range(8):
                for hf in range(2):
                    s = wi % NWS
                    wi += 1
                    S.dma(f"Ow{s}", wv[s], w_query[c * 128:(c + 1) * 128, hf * 1024:(hf + 1) * 1024], writes=[f"wst{s}"])
                    cp(castrot.next(), wq[:, c, hf * 1024:(hf + 1) * 1024], wv[s], [f"wst{s}"], [f"wq{c}"])
            skb = sb("O_skb", [128, 16, 128], BF16)
            skT = sb("O_skT", [128, 16, 128], BF16)
            for hp in range(16):
                s = wi % NWS
                wi += 1
                S.dma(f"Ow{s}", wv[s][:, 0:128], sub_keys[hp * 128:(hp + 1) * 128, :], writes=[f"wst{s}"])
                cp("dve", skb[:, hp, :], wv[s][:, 0:128], [f"wst{s}"], ["skb"])
            for half in range(2):
                for k in range(8):
                    hp = half * 8 + k
                    tr(psb(half)[:, k * 128:(k + 1) * 128], skb[:, hp, :], ident[:], ["skb", "ident"], [f"ps{half}"])
                cp("dve", skT[:, half * 8:(half + 1) * 8, :], psb(half).rearrange("p (k n) -> p k n", n=128), [f"ps{half}"], ["skT"])

            TB = 8
            h2t = [sb(f"O_h2t{k}", [128, 8, 128], BF16) for k in range(2)]
            qT = [sb(f"O_qT{k}", [128, 16, 128], BF16) for k in range(2)]
            sc = [sb(f"O_sc{k}", [128, 16, 128], F32) for k in range(2)]
            scw = sb("O_scw", [128, 16, 128], F32)
            top = sb("O_top", [128, 16, 16], F32)
            topi = sb("O_topi", [128, 16, 16], U32)
            topf = sb("O_topf", [128, 16, 16], F32)
            cand = sb("O_cand", [128, 8, 256], F32)
            candw = sb("O_candw", [128, 8, 256], F32)
            best = sb("O_best", [128, 8, 16], F32)
            posi = sb("O_posi", [128, 8, 16], U32)
            k1u = sb("O_k1u", [128, 8, 16], U32)
            posf = sb("O_posf", [128, 8, 16], F32)
            k0f = sb("O_k0f", [128, 8, 16], F32)
            k1f = sb("O_k1f", [128, 8, 16], F32)
            i0f = sb("O_i0f", [128, 128], F32)
            i1f = sb("O_i1f", [128, 128], F32)
            gat = sb("O_gat", [128, 128], F32)
            ssum = sb("O_ssum", [128, 8], F32)
            pkT = [sb(f"O_pkT{k}", [128, 3, 128], BF16) for k in range(2)]
            pk32 = [sb(f"O_pk32{k}", [128, 2, 128], F32) for k in range(2)]
            NOH = 3
            oh1 = [sb(f"O_oh1{k}", [128, TB, 128], BF16) for k in range(NOH)]
            oh0g = [sb(f"O_oh0g{k}", [128, TB, 128], BF16) for k in range(NOH)]
            Gsb = [sb(f"O_G{k}", [128, 128, 128], BF16) for k in range(2)]
            grot = Rot([5, 6, 7])
            TRB = 4

            def stage_A(ti):
                hs = ti % 2
                S.dma(f"Oh{hs}", h2t[hs][:], h2Td[:, :, ti * 128:(ti + 1) * 128], reads=["h2Td"], writes=[f"h2t{hs}"])
                for q4 in range(4):
                    bk = q4 % 2
                    for k in range(4):
                        hp = q4 * 4 + k
                        o = psf(bk)[:, k * 128:(k + 1) * 128]
                        for c in range(8):
                            mm(o, wq[:, c, hp * 128:(hp + 1) * 128], h2t[hs][:, c, :], c == 0, c == 7, [f"wq{c}", f"h2t{hs}"], [f"ps{bk}"])
                        if k % 2 == 1:
                            yield
                    cp("act", qT[hs][:, q4 * 4:(q4 + 1) * 4, :], psf(bk).rearrange("p (k t) -> p k t", t=128), [f"ps{bk}"], [f"qT{hs}_{q4}"])
                for q4 in range(4):
                    bk = 2 + q4 % 2
                    for k in range(4):
                        hp = q4 * 4 + k
                        mm(psf(bk)[:, k * 128:(k + 1) * 128], qT[hs][:, hp, :], skT[:, hp, :], True, True, [f"qT{hs}_{q4}", "skT"], [f"ps{bk}"])
                    cp("act", sc[hs][:, q4 * 4:(q4 + 1) * 4, :], psf(bk).rearrange("p (k n) -> p k n", n=128), [f"ps{bk}"], [f"sc{hs}_{q4}"])
                    yield

            def stage_B(ti):
                hs = ti % 2
                for hp in range(16):
                    S.op("dve", lambda E, hp=hp, hs=hs: E.max(out=top[:, hp, 0:8], in_=sc[hs][:, hp, :]), [f"sc{hs}_{hp // 4}"], [f"tA{hp}"])
                yield
                for hp in range(16):
                    S.op("dve", lambda E, hp=hp, hs=hs: E.max_index(out=topi[:, hp, 0:8], in_max=top[:, hp, 0:8], in_values=sc[hs][:, hp, :]),
                         [f"sc{hs}_{hp // 4}", f"tA{hp}"], [f"tiA{hp}"])
                    S.op("dve", lambda E, hp=hp, hs=hs: E.match_replace(out=scw[:, hp, :], in_to_replace=top[:, hp, 0:8], in_values=sc[hs][:, hp, :], imm_value=-1e30),
                         [f"sc{hs}_{hp // 4}", f"tA{hp}"], [f"scw{hp}"])
                    if hp % 8 == 7:
                        yield
                for hp in range(16):
                    S.op("dve", lambda E, hp=hp: E.max(out=top[:, hp, 8:16], in_=scw[:, hp, :]), [f"scw{hp}"], [f"tB{hp}"])
                yield
                for hp in range(16):
                    S.op("dve", lambda E, hp=hp: E.max_index(out=topi[:, hp, 8:16], in_max=top[:, hp, 8:16], in_values=scw[:, hp, :]),
                         [f"scw{hp}", f"tB{hp}"], [f"tiB{hp}"])
                TOPK = [f"tA{hp}" for hp in range(16)] + [f"tB{hp}" for hp in range(16)]
                TOPI = [f"tiA{hp}" for hp in range(16)] + [f"tiB{hp}" for hp in range(16)]
                top5 = top[:].rearrange("p (h two) k -> p h two k", two=2)
                topf5 = topf[:].rearrange("p (h two) k -> p h two k", two=2)
                tt("dve", cand[:].rearrange("p h (a b) -> p h a b", b=16),
                   top5[:, :, 0, :].unsqueeze(3).to_broadcast([128, 8, 16, 16]),
                   top5[:, :, 1, :].unsqueeze(2).to_broadcast([128, 8, 16, 16]), ALU.add, TOPK, ["cand"])
                cp("dve", topf[:], topi[:], TOPI, ["topf"])
                yield
                for h in range(8):
                    S.op("dve", lambda E, h=h: E.max(out=best[:, h, 0:8], in_=cand[:, h, :]), ["cand"], [f"bA{h}"])
                yield
                for h in range(8):
                    S.op("dve", lambda E, h=h: E.max_index(out=posi[:, h, 0:8], in_max=best[:, h, 0:8], in_values=cand[:, h, :]), ["cand", f"bA{h}"], [f"pA{h}"])
                    S.op("dve", lambda E, h=h: E.match_replace(out=candw[:, h, :], in_to_replace=best[:, h, 0:8], in_values=cand[:, h, :], imm_value=-1e30),
                         ["cand", f"bA{h}"], [f"cw{h}"])
                yield
                for h in range(8):
                    S.op("dve", lambda E, h=h: E.max(out=best[:, h, 8:16], in_=candw[:, h, :]), [f"cw{h}"], [f"bB{h}"])
                yield
                for h in range(8):
                    S.op("dve", lambda E, h=h: E.max_index(out=posi[:, h, 8:16], in_max=best[:, h, 8:16], in_values=candw[:, h, :]), [f"cw{h}", f"bB{h}"], [f"pB{h}"])
                BEST = [f"bA{h}" for h in range(8)] + [f"bB{h}" for h in range(8)]
                POS = [f"pA{h}" for h in range(8)] + [f"pB{h}" for h in range(8)]
                gv = gat[:].rearrange("p (h k) -> p h k", k=16)
                tt("dve", gv, best[:], best[:, :, 0:1].to_broadcast([128, 8, 16]), ALU.subtract, BEST, ["gat"])
                act(gat[:], gat[:], AF.Exp, ["gat"], ["gat"])
                S.op("dve", lambda E: E.tensor_single_scalar(out=k1u[:], in_=posi[:], scalar=15, op=ALU.bitwise_and), POS, ["k1u"])
                cp("dve", posf[:], posi[:], POS, ["posf"])
                yield
                cp("dve", k1f[:], k1u[:], ["k1u"], ["k1f"])
                tt("dve", k0f[:], posf[:], k1f[:], ALU.subtract, ["posf", "k1f"], ["k0f"])
                ts("dve", k0f[:], k0f[:], 1.0 / 16.0, None, ALU.mult, None, ["k0f"], ["k0f"])
                S.op("dve", lambda E: E.tensor_reduce(out=ssum[:], in_=gat[:].rearrange("p (h k) -> p h k", k=16), axis=AX.X, op=ALU.add), ["gat"], ["ssum"])
                recip(ssum[:], ssum[:], ["ssum"], ["ssum"])
                yield
                io4 = iota16[:].unsqueeze(1).unsqueeze(1).to_broadcast([128, 8, 16, 16])
                i0v = i0f[:].rearrange("p (h k) -> p h k", k=16)
                i1v = i1f[:].rearrange("p (h k) -> p h k", k=16)
                tt("dve", eq[1][:], k1f[:].unsqueeze(3).to_broadcast([128, 8, 16, 16]), io4, ALU.is_equal, ["k1f", "iota16"], ["eq1"])
                tt("dve", eq[1][:], eq[1][:], topf5[:, :, 1, :].unsqueeze(2).to_broadcast([128, 8, 16, 16]), ALU.mult, ["eq1", "topf"], ["eq1"])
                yield
                tt("dve", eq[0][:], k0f[:].unsqueeze(3).to_broadcast([128, 8, 16, 16]), io4, ALU.is_equal, ["k0f", "iota16"], ["eq0"])
                tt("dve", eq[0][:], eq[0][:], topf5[:, :, 0, :].unsqueeze(2).to_broadcast([128, 8, 16, 16]), ALU.mult, ["eq0", "topf"], ["eq0"])
                tt("dve", gv, gv, ssum[:].unsqueeze(2).to_broadcast([128, 8, 16]), ALU.mult, ["gat", "ssum"], ["gat"])
                yield
                S.op("dve", lambda E: E.tensor_reduce(out=i1v, in_=eq[1][:], axis=AX.X, op=ALU.add), ["eq1"], ["i1f"])
                yield
                S.op("dve", lambda E: E.tensor_reduce(out=i0v, in_=eq[0][:], axis=AX.X, op=ALU.add), ["eq0"], ["i0f"])
                for k, (src, sname) in enumerate(((i0f, "i0f"), (i1f, "i1f"), (gat, "gat"))):
                    S.op("pe", lambda E, k=k, src=src: E.transpose(psf(TRB)[:, k * 128:(k + 1) * 128], src[:], identf[:]), [sname, "identf"], [f"ps{TRB}"])
                cp("act", pkT[hs][:], psf(TRB)[:, 0:384].rearrange("p (k t) -> p k t", t=128), [f"ps{TRB}"], [f"pkT{hs}"])
                cp("act", pk32[hs][:, 0, :], psf(TRB)[:, 0:128], [f"ps{TRB}"], [f"pk32{hs}"])
                cp("act", pk32[hs][:, 1, :], psf(TRB)[:, 256:384], [f"ps{TRB}"], [f"pk32{hs}"])
                yield

            ohc = {"n": 0}

            def stage_C(ti):
                hs = ti % 2
                gs = ti % 2
                pk = f"pkT{hs}"
                iob = iota[:].unsqueeze(1).to_broadcast([128, TB, 128])
                NB = 128 // TB

                for b in range(NB):
                    os_ = (ohc["n"] + b) % NOH
                    t0 = b * TB
                    tt("dve", oh1[os_][:], iob, pkT[hs][:, 1, t0:t0 + TB].unsqueeze(2).to_broadcast([128, TB, 128]), ALU.is_equal, ["iota", pk], [f"oh1{os_}"])
                    for tloc in range(TB):
                        ts("dve", oh0g[os_][:, tloc, :], iota[:], pk32[hs][:, 0, t0 + tloc:t0 + tloc + 1], pk32[hs][:, 1, t0 + tloc:t0 + tloc + 1],
                           ALU.is_equal, ALU.mult, ["iota", f"pk32{hs}"], [f"oh0g{os_}_{tloc}"])
                    for t4 in range(0, TB, 4):
                        gb = grot.next()
                        for tq in range(4):
                            tloc = t4 + tq
                            mm(psf(gb)[:, tq:509 + tq:4], oh1[os_][:, tloc, :], oh0g[os_][:, tloc, :], True, True,
                               [f"oh1{os_}", f"oh0g{os_}_{tloc}"], [f"ps{gb}"])
                        tg = t0 + t4
                        cp("act", Gsb[gs][:, :, tg:tg + 4], psf(gb).rearrange("j (i t) -> j i t", t=4), [f"ps{gb}"], [f"G{gs}_{tg}"])
                    yield
                ohc["n"] += NB
                GK = [f"G{gs}_{tg}" for tg in range(0, 128, 4)]
                for i0 in range(0, 128, 8):
                    S.dma(f"Og{gs}", Gd[i0:i0 + 8, :, ti * 128:(ti + 1) * 128].rearrange("i j t -> j i t"), Gsb[gs][:, i0:i0 + 8, :],
                          reads=GK, writes=["Gd"])
                yield

            for _ in stage_A(0):
                pass
            for t in range(NT + 1):
                gens = []
                if t < NT:
                    gB = stage_B(t)
                    gens += [gB, gB]
                if t >= 1:
                    gens.append(stage_C(t - 1))
                if t + 1 < NT:
                    gens.append(stage_A(t + 1))
                while gens:
                    for g_ in list(gens):
                        if g_ not in gens:
                            continue
                        try:
                            next(g_)
                        except StopIteration:
                            while g_ in gens:
                                gens.remove(g_)

        def phase_E(ph):
            sb = lambda n, s, d: ph.enter_context(nc.sbuf_tensor(n, s, d))
            TG = 1024
            NG = SEQ // TG
            SBK = 8
            NR = 16
            NSB = 128 // SBK
            NSG = TG // 256
            gfin = sb("E_gfin", [128, DM], F32)
            S.dma("c0", gfin[:], g_fin.partition_broadcast(128), writes=["gfin"])
            x1s = [sb(f"E_x1{k}", [128, TG // 128, DM], F32) for k in range(2)]
            h2Ts = [sb(f"E_h2T{k}", [128, 8, TG], BF16) for k in range(2)]
            UTs = [sb(f"E_UT{k}", [128, 8 * 128], BF16) for k in range(NR)]
            Vbs = [sb(f"E_Vb{k}", [128, DM], BF16) for k in range(NR)]
            Gts = [sb(f"E_Gt{k}", [128, TG], BF16) for k in range(NR)]
            NW = 4
            gel = [sb(f"E_gel{k}", [128, 256], BF16) for k in range(NW)]
            WT = [sb(f"E_WT{k}", [128, 256], BF16) for k in range(NW)]
            junk = sb("E_junk", [128, DM], BF16)
            st1 = [sb(f"E_st{k}", [128, 4], F32) for k in range(2)]
            ost = [sb(f"E_ost{k}", [128, DM], F32) for k in range(1)]
            arot = Rot([0, 1, 6, 7])
            grot = Rot(list(range(NW)))
            wrot = Rot(["dve", "pool"])
            D = 2

            SBL = [(g, sbk) for g in range(NG) for sbk in range(NSB)]

            def load_sb(k):
                g, sbk = SBL[k]
                tok0 = g * TG
                for ii in range(SBK):
                    i = sbk * SBK + ii
                    r = (k * SBK + ii) % NR
                    S.dma_group(f"El{r}", [
                        (UTs[r][:], UTd[i], ["UTd"], [f"UT{r}"], {}),
                        (Vbs[r][:], Vbd[i * 128:(i + 1) * 128, :], ["Vbd"], [f"Vb{r}"], {}),
                        (Gts[r][:], Gd[i, :, tok0:tok0 + TG], ["Gd"], [f"Gt{r}"], {}),
                    ])

            def load_group(g):
                tok0 = g * TG
                gb = g % 2
                S.dma_group(f"Ex1{gb}", [
                    (x1s[gb][:, tl, :], x1d[tok0 + tl * 128:tok0 + (tl + 1) * 128, :], ["x1d"], [f"x1{gb}_{tl}_0", f"x1{gb}_{tl}_1"], {}) for tl in range(TG // 128)
                ] + [(h2Ts[gb][:], h2Td[:, :, tok0:tok0 + TG], ["h2Td"], [f"h2T{gb}"], {})])

            its = []
            for k, (g, sbk) in enumerate(SBL):
                for sg in range(NSG):
                    for ii in range(SBK):
                        its.append(dict(k=k, g=g, sbk=sbk, sg=sg, ii=ii,
                                        sb_first=(sg == 0 and ii == D), grp_first=(sbk == 0 and sg == 0 and ii == 0),
                                        grp_last=(sbk == NSB - 1 and sg == NSG - 1 and ii == SBK - 1)))
            S.dma_group("Ex10", [(h2Ts[0][:], h2Td[:, :, 0:TG], ["h2Td"], ["h2T0"], {})])
            load_sb(0)
            S.dma_group("Ex10", [
                (x1s[0][:, tl, :], x1d[tl * 128:(tl + 1) * 128, :], ["x1d"], [f"x10_{tl}_0", f"x10_{tl}_1"], {}) for tl in range(TG // 128)
            ])

            def stage1(it):
                k, g, sg, ii = it["k"], it["g"], it["sg"], it["ii"]
                if it["sb_first"] and it["sbk"] == 0 and g + 1 < NG:
                    load_group(g + 1)
                if it["sb_first"] and k + 1 < len(SBL):
                    load_sb(k + 1)
                x1 = x1s[g % 2]
                h2T = h2Ts[g % 2]
                gb = g % 2
                r = (k * SBK + ii) % NR
                ab = arot.next()
                akey = f"ps{ab}"
                ao = psf(ab)[:, 0:256]
                for c in range(8):
                    mm(ao, UTs[r][:, c * 128:(c + 1) * 128], h2T[:, c, sg * 256:(sg + 1) * 256], c == 0, c == 7, [f"UT{r}", f"h2T{gb}"], [akey])
                gsl = grot.next()
                it["gsl"] = gsl
                act(gel[gsl][:], ao, AF.Gelu, [akey], [f"gel{gsl}"])
                tt(wrot.next(), WT[gsl][:], gel[gsl][:], Gts[r][:, sg * 256:(sg + 1) * 256], ALU.mult, [f"gel{gsl}", f"Gt{r}"], [f"WT{gsl}"])

            def stage2(it):
                k, g, sg, ii = it["k"], it["g"], it["sg"], it["ii"]
                r = (k * SBK + ii) % NR
                gsl = it["gsl"]
                x1 = x1s[g % 2]
                gb = g % 2
                for tl in range(2):
                    for hf in range(2):
                        b = 2 + tl * 2 + hf
                        mm(psf(b), WT[gsl][:, tl * 128:(tl + 1) * 128], Vbs[r][:, hf * 512:(hf + 1) * 512], ii == 0, ii == SBK - 1,
                           [f"WT{gsl}", f"Vb{r}"], [f"ps{b}"])
                if ii == SBK - 1:
                    for tl in range(2):
                        tix = sg * 2 + tl
                        for hf in range(2):
                            b = 2 + tl * 2 + hf
                            xk = f"x1{gb}_{tix}_{hf}"
                            tt("dve", x1[:, tix, hf * 512:(hf + 1) * 512], x1[:, tix, hf * 512:(hf + 1) * 512], psf(b), ALU.add, [xk, f"ps{b}"], [xk])
                if it["grp_last"]:
                    tok0 = g * TG
                    for tl in range(TG // 128):
                        s = tl % 2
                        XK = [f"x1{gb}_{tl}_0", f"x1{gb}_{tl}_1"]
                        S.op("act", lambda E, s=s: E.memzero(st1[s][:, 0:1]), [], [f"ss{s}"])
                        act(junk[:], x1[:, tl, :], AF.Square, XK, [f"junk{s}", f"ss{s}"], accum=st1[s][:, 0:1])
                        act(st1[s][:, 1:2], st1[s][:, 0:1], AF.Sqrt, [f"ss{s}"], [f"sd{s}"], scale=1.0 / DM, bias=EPS)
                        recip(st1[s][:, 2:3], st1[s][:, 1:2], [f"sd{s}"], [f"rs{s}"])
                        stt("dve", ost[0][:], x1[:, tl, :], st1[s][:, 2:3], gfin[:], ALU.mult, ALU.mult, XK + [f"rs{s}", "gfin"], ["ost0"])
                        S.dma("Eo0", out[tok0 + tl * 128:tok0 + (tl + 1) * 128, :], ost[0][:], reads=["ost0"], writes=["out"])

            NI = len(its)
            for idx in range(NI + D):
                if idx < NI:
                    stage1(its[idx])
                if idx >= D and not its[idx - D].get("done"):
                    stage2(its[idx - D])
                    its[idx - D]["done"] = True

        phases = [("P", phase_P), ("A", phase_A), ("M", phase_M), ("X", phase_X), ("O", phase_O), ("E", phase_E)]
        only = (debug or {}).get("_only")
        ustack = ExitStack()
        ugen = None
        UB = {}
        for name, fn in phases:
            if only is None or name in only:
                if name == "A" and (only is None or "U" in only):
                    UB.update(u_alloc(ustack))
                    ugen = u_gen(UB)
                with ExitStack() as ph:
                    if name in ("A", "M"):
                        fn(ph, ugen)
                    else:
                        fn(ph)
                    S.barrier()
                if name == "M":
                    ustack.close()
            if name == stop_after:
                break
        ustack.close()
        S.barrier()
        with nc.Block() as block:
            S.replay(block)
        build_program.stats = (S.n_ins, S.n_wait, dict(S.cnt))
    return nc


_CONSTS = None


def _layout_inputs(inputs):
    global _CONSTS
    if _CONSTS is None:
        _CONSTS = _host_consts()
    f = lambda a: np.ascontiguousarray(np.asarray(a, dtype=np.float32))
    shared = {
        "attn_norm_g": f(inputs["attn_norm_g"]).reshape(DM),
        "w_in": f(inputs["w_in"]).reshape(DM, 2080),
        "mla_q_norm_g": f(inputs["mla_q_norm_g"]).reshape(256),
        "mla_kv_norm_g": f(inputs["mla_kv_norm_g"]).reshape(256),
        "w_uq": f(inputs["w_uq"]).reshape(256, 768),
        "w_uk": f(inputs["w_uk"]).reshape(256, 512),
        "w_uv": f(inputs["w_uv"]).reshape(256, 512),
        "w_o": f(inputs["w_o"]).reshape(DM, DM),
        "ffn_norm_g": f(inputs["ffn_norm_g"]).reshape(DM),
        "peer_w_query": f(inputs["peer_w_query"]).reshape(DM, 2048),
        "peer_sub_keys": f(inputs["peer_sub_keys"]).reshape(16 * 128, 128),
        "peer_u": f(inputs["peer_u"]).reshape(16384, DM),
        "peer_v": f(inputs["peer_v"]).reshape(16384, DM),
        "final_norm_g": f(inputs["final_norm_g"]).reshape(DM),
    }
    shared.update(_CONSTS)
    return shared


def kernel(**inputs):
    shared = _layout_inputs(inputs)
    xs = np.asarray(inputs["x"], dtype=np.float32)
    n = xs.shape[0]
    nc = build_program()
    in_maps = []
    for b in range(n):
        m = dict(shared)
        m["x"] = np.ascontiguousarray(xs[b])
        in_maps.append(m)
    res = run_bass_kernel_spmd(nc, in_maps, core_ids=list(range(n)))
    return np.stack([np.asarray(r["out"], dtype=np.float32) for r in res.results], axis=0)
```

```python
import numpy as np
import ml_dtypes
from contextlib import ExitStack
import concourse.bass as bass
import concourse.mybir as mybir
from concourse.bass_utils import run_bass_kernel_spmd

F32 = mybir.dt.float32
BF16 = mybir.dt.bfloat16
U32 = mybir.dt.uint32
ALU = mybir.AluOpType
AF = mybir.ActivationFunctionType
AX = mybir.AxisListType

ENGS = ["pe", "act", "dve", "pool", "sp"]
SAME_ENGINE_SYNC = {"pool", "act", "dve"}
SAME_ENGINE_DIST = 8

SEQ = 4096
DM = 1024
NT = SEQ // 128
EPS = 1e-6
THETA = 10000.0
NEXP_BLK = 128


class Sched:
    def __init__(self, nc, stack):
        self.nc = nc
        self.stack = stack
        self.q = {e: [] for e in ENGS}
        self.cnt = {e: 0 for e in ENGS}
        self.known = {e: {} for e in ENGS}
        self.lastw = {}
        self.readers = {}
        self.esem = {e: stack.enter_context(nc.semaphore("es_" + e)) for e in ENGS}
        self.dsem = {}
        self.dcnt = {}
        self.n_ins = 0
        self.n_wait = 0

    def _emit_waits(self, eng, deps):
        need = {}
        for key, val in deps:
            if key == eng and (eng not in SAME_ENGINE_SYNC or self.cnt[eng] - val >= SAME_ENGINE_DIST):
                continue
            if self.known[eng].get(key, 0) >= val:
                continue
            if need.get(key, 0) < val:
                need[key] = val
        for key, val in need.items():
            self.known[eng][key] = val
            sem = self.esem[key] if key in self.esem else self.dsem[key]
            self.q[eng].append(("w", sem, val))
            self.n_wait += 1

    def _deps(self, reads, writes):
        deps = []
        for b in reads:
            deps.extend(self.lastw.get(b, {}).items())
        for b in writes:
            deps.extend(self.lastw.get(b, {}).items())
            deps.extend(self.readers.get(b, {}).items())
        return deps

    def _record(self, key, val, reads, writes):
        for b in reads:
            r = self.readers.setdefault(b, {})
            if r.get(key, 0) < val:
                r[key] = val
        for b in writes:
            if self.readers.get(b):
                self.lastw[b] = {key: val}
                self.readers[b] = {}
            else:
                w = self.lastw.setdefault(b, {})
                if w.get(key, 0) < val:
                    w[key] = val

    def op(self, eng, fn, reads=(), writes=()):
        self._emit_waits(eng, self._deps(reads, writes))
        self.cnt[eng] += 1
        self.q[eng].append(("i", fn))
        self.n_ins += 1
        self._record(eng, self.cnt[eng], reads, writes)

    def dma(self, stream, out, in_, reads=(), writes=(), queue="sp", **kw):
        if stream not in self.dsem:
            self.dsem[stream] = self.stack.enter_context(self.nc.semaphore("ds_" + stream))
            self.dcnt[stream] = 0
        self._emit_waits(queue, self._deps(reads, writes))
        self.dcnt[stream] += 16
        self.q[queue].append(("d", out, in_, self.dsem[stream], kw))
        self.n_ins += 1
        self._record(stream, self.dcnt[stream], reads, writes)

    def dma_group(self, stream, items, queue="sp"):
        if stream not in self.dsem:
            self.dsem[stream] = self.stack.enter_context(self.nc.semaphore("ds_" + stream))
            self.dcnt[stream] = 0
        deps = []
        for (o, i, R, W, kw) in items:
            deps.extend(self._deps(R, W))
        self._emit_waits(queue, deps)
        for (o, i, R, W, kw) in items:
            self.dcnt[stream] += 16
            self.q[queue].append(("d", o, i, self.dsem[stream], kw))
            self.n_ins += 1
        for (o, i, R, W, kw) in items:
            self._record(stream, self.dcnt[stream], R, W)

    def wait_all(self, eng):
        deps = [(e, self.cnt[e]) for e in ENGS if self.cnt[e] > 0 and e != eng]
        deps += [(s, v) for s, v in self.dcnt.items() if v > 0]
        self._emit_waits(eng, deps)

    def barrier(self):
        for e in ENGS:
            self.wait_all(e)

    def replay(self, block):
        names = {"pe": "tensor", "act": "scalar", "dve": "vector", "pool": "gpsimd", "sp": "sync"}

        def make(e):
            def body(E):
                sem = self.esem[e]
                for item in self.q[e]:
                    if item[0] == "w":
                        E.wait_ge(item[1], item[2])
                    elif item[0] == "i":
                        item[1](E).then_inc(sem, 1)
                    else:
                        _, out, in_, dsem, kw = item
                        E.dma_start(out=out, in_=in_, **kw).then_inc(dsem, 16)
            return body

        for e in ENGS:
            getattr(block, names[e])(make(e))


class Rot:
    def __init__(self, items):
        self.items = list(items)
        self.i = 0

    def next(self):
        v = self.items[self.i % len(self.items)]
        self.i += 1
        return v


def _host_consts():
    t = np.arange(SEQ, dtype=np.float32)
    invA = (1.0 / (THETA ** (np.arange(0, 64, 2, dtype=np.float32) / 64.0))).astype(np.float32)
    invM = (1.0 / (THETA ** (np.arange(0, 32, 2, dtype=np.float32) / 32.0))).astype(np.float32)
    angA = (t[None, :] * invA[:, None]).astype(np.float32)
    angM = (t[None, :] * invM[:, None]).astype(np.float32)
    tabs = np.zeros((4, 128, SEQ), np.float32)
    for p in range(128):
        f = p % 32
        tabs[0, p] = np.cos(angA[f])
        tabs[1, p] = np.sin(angA[f]) * (1.0 if (p % 64) < 32 else -1.0)
    for q in range(32):
        f = q % 16
        tabs[2, 64 + q] = np.cos(angM[f])
        tabs[3, 64 + q] = np.sin(angM[f]) * (-1.0 if q < 16 else 1.0)
    k = np.arange(128)[:, None]
    qq = np.arange(128)[None, :]
    mask2 = np.concatenate([(k >= qq), (k <= qq)], axis=1).astype(np.float32)
    return {
        "c_tabs": tabs.astype(ml_dtypes.bfloat16),
        "c_ident": np.eye(128, dtype=np.float32).astype(ml_dtypes.bfloat16),
        "c_identf": np.eye(128, dtype=np.float32),
        "c_mask2": mask2.astype(ml_dtypes.bfloat16),
        "c_iota": np.tile(np.arange(128, dtype=np.float32)[None, :], (128, 1)).astype(ml_dtypes.bfloat16),
        "c_iota16": np.tile(np.arange(16, dtype=np.float32)[None, :], (128, 1)),
    }


def build_program(debug=None):
    nc = bass.Bass("TRN2", target_bir_lowering=False)

    def din(name, shape, dt=F32):
        return nc.dram_tensor(name, list(shape), dt, kind="ExternalInput").ap()

    def dscr(name, shape, dt):
        kind = "ExternalOutput" if (debug and name in debug) else "Internal"
        return nc.dram_tensor(name, list(shape), dt, kind=kind).ap()

    x = din("x", [SEQ, DM])
    g_attn = din("attn_norm_g", [DM])
    w_in = din("w_in", [DM, 2080])
    g_q = din("mla_q_norm_g", [256])
    g_kv = din("mla_kv_norm_g", [256])
    w_uq = din("w_uq", [256, 768])
    w_uk = din("w_uk", [256, 512])
    w_uv = din("w_uv", [256, 512])
    w_o = din("w_o", [DM, DM])
    g_ffn = din("ffn_norm_g", [DM])
    w_query = din("peer_w_query", [DM, 2048])
    sub_keys = din("peer_sub_keys", [16 * 128, 128])
    peer_u = din("peer_u", [16384, DM])
    peer_v = din("peer_v", [16384, DM])
    g_fin = din("final_norm_g", [DM])
    c_tabs = din("c_tabs", [4, 128, SEQ], BF16)
    c_ident = din("c_ident", [128, 128], BF16)
    c_identf = din("c_identf", [128, 128], F32)
    c_mask2 = din("c_mask2", [128, 256], BF16)
    c_iota = din("c_iota", [128, 128], BF16)
    c_iota16 = din("c_iota16", [128, 16], F32)
    out = nc.dram_tensor("out", [SEQ, DM], F32, kind="ExternalOutput").ap()

    qkT = dscr("qkT", [8, 128, SEQ], BF16)
    vA = dscr("vA", [SEQ, 768], BF16)
    mq = dscr("mq", [8, 96, SEQ], BF16)
    mk = dscr("mk", [8, 96, SEQ], BF16)
    vM = dscr("vM", [SEQ, 768], BF16)
    mixT = dscr("mixT", [8, 128, SEQ], BF16)
    x1d = dscr("x1d", [SEQ, DM], F32)
    h2Td = dscr("h2Td", [128, 8, SEQ], BF16)
    Gd = dscr("Gd", [128, 128, SEQ], BF16)
    UTd = dscr("UTd", [128, 128, 8 * 128], BF16)
    Vbd = dscr("Vbd", [16384, DM], BF16)

    stop_after = (debug or {}).get("_stop", "E")

    with ExitStack() as top:
        S = Sched(nc, top)
        ps = [top.enter_context(nc.psum_tensor(f"ps{k}", [128, 512], F32)) for k in range(8)]

        def psf(k):
            return ps[k][:]

        def psb(k):
            return ps[k][:].bitcast(BF16)

        def mm(o, l, r, st, sp_, R, W):
            S.op("pe", lambda E, o=o, l=l, r=r, st=st, sp_=sp_: E.matmul(o, lhsT=l, rhs=r, start=st, stop=sp_), R, W)

        def tr(o, i, idn, R, W):
            S.op("pe", lambda E, o=o, i=i, idn=idn: E.transpose(o, i, idn), R, W)

        def tt(eng, o, a, b, op, R, W):
            S.op(eng, lambda E, o=o, a=a, b=b, op=op: E.tensor_tensor(out=o, in0=a, in1=b, op=op), R, W)

        def ts(eng, o, a, s1, s2, op0, op1, R, W):
            if op1 is None:
                S.op(eng, lambda E, o=o, a=a, s1=s1, op0=op0: E.tensor_scalar(out=o, in0=a, scalar1=s1, scalar2=None, op0=op0), R, W)
            else:
                S.op(eng, lambda E, o=o, a=a, s1=s1, s2=s2, op0=op0, op1=op1: E.tensor_scalar(out=o, in0=a, scalar1=s1, scalar2=s2, op0=op0, op1=op1), R, W)

        def stt(eng, o, a, sc, b, op0, op1, R, W):
            S.op(eng, lambda E, o=o, a=a, sc=sc, b=b, op0=op0, op1=op1: E.scalar_tensor_tensor(out=o, in0=a, scalar=sc, in1=b, op0=op0, op1=op1), R, W)

        def cp(eng, o, i, R, W):
            if eng == "act":
                S.op(eng, lambda E, o=o, i=i: E.copy(out=o, in_=i), R, W)
            else:
                S.op(eng, lambda E, o=o, i=i: E.tensor_copy(out=o, in_=i), R, W)

        def act(o, i, func, R, W, scale=1.0, bias=None, accum=None):
            kw = {}
            if bias is not None:
                kw["bias"] = bias
            if accum is not None:
                kw["accum_out"] = accum
            S.op("act", lambda E, o=o, i=i, func=func, scale=scale, kw=kw: E.activation(out=o, in_=i, func=func, scale=scale, **kw), R, W)

        def recip(o, i, R, W):
            S.op("dve", lambda E, o=o, i=i: E.reciprocal(out=o, in_=i), R, W)

        def recipf(o, i, R, W):
            S.op("dve", lambda E, o=o, i=i: E.reciprocal_approx_fast(out=o, in_=i), R, W)

        def memset(eng, o, v, W):
            S.op(eng, lambda E, o=o, v=v: E.memset(o, v), (), W)

        castrot = Rot(["act", "dve", "pool"])

        def phase_U(ph):
            sb = lambda n, s, d: ph.enter_context(nc.sbuf_tensor(n, s, d))
            ident = sb("U_ident", [128, 128], BF16)
            S.dma("c0", ident[:], c_ident, writes=["ident"])
            NS = 3
            ust = [sb(f"U_ust{k}", [128, DM], F32) for k in range(NS)]
            vst = [sb(f"U_vst{k}", [128, DM], F32) for k in range(NS)]
            ubf = [sb(f"U_ubf{k}", [128, DM], BF16) for k in range(NS)]
            vbf = [sb(f"U_vbf{k}", [128, DM], BF16) for k in range(NS)]
            utl = [sb(f"U_utl{k}", [128, 8 * 128], BF16) for k in range(NS)]
            for i in range(128):
                s = i % NS
                pb = i % 2
                S.dma_group(f"Ul{s}", [
                    (ust[s][:], peer_u[i * 128:(i + 1) * 128, :], [], [f"ust{s}"], {}),
                    (vst[s][:], peer_v[i * 128:(i + 1) * 128, :], [], [f"vst{s}"], {}),
                ])
                cp(castrot.next(), ubf[s][:], ust[s][:], [f"ust{s}"], [f"ubf{s}"])
                cp(castrot.next(), vbf[s][:], vst[s][:], [f"vst{s}"], [f"vbf{s}"])
                for c in range(8):
                    tr(psb(pb)[:, c * 128:(c + 1) * 128], ubf[s][:, c * 128:(c + 1) * 128], ident[:], [f"ubf{s}", "ident"], [f"ps{pb}"])
                cp("dve" if i % 2 == 0 else "act", utl[s][:], psb(pb), [f"ps{pb}"], [f"utl{s}"])
                S.dma_group(f"Us{s}", [
                    (UTd[i], utl[s][:], [f"utl{s}"], ["UTd"], {}),
                    (Vbd[i * 128:(i + 1) * 128, :], vbf[s][:], [f"vbf{s}"], ["Vbd"], {}),
                ])

        def phase_P(ph):
            sb = lambda n, s, d: ph.enter_context(nc.sbuf_tensor(n, s, d))
            ident = sb("P_ident", [128, 128], BF16)
            ones = sb("P_ones", [128, 128], BF16)
            memset("dve", ones[:], 1.0, ["ones"])
            win = sb("P_win", [128, 8, 2080], BF16)
            winsw = sb("P_winsw", [128, 8, 96], BF16)
            wuq = sb("P_wuq", [128, 2, 768], BF16)
            wuqsw = sb("P_wuqsw", [128, 2, 768], BF16)
            wuk = sb("P_wuk", [128, 2, 512], BF16)
            wuv = sb("P_wuv", [128, 2, 512], BF16)
            wst = [sb(f"P_wst{k}", [128, 2080], F32) for k in range(4)]
            g1 = sb("P_g1", [128, 8], F32)
            gq = sb("P_gq", [128, 2], F32)
            gkv = sb("P_gkv", [128, 2], F32)
            nck = {"allow_slow_non_contiguous": True}
            S.dma_group("c0", [
                (ident[:], c_ident, [], ["ident"], {}),
                (g1[:], g_attn.rearrange("(c p) -> p c", p=128), [], ["g1"], nck),
                (gq[:], g_q.rearrange("(c p) -> p c", p=128), [], ["gq"], nck),
                (gkv[:], g_kv.rearrange("(c p) -> p c", p=128), [], ["gkv"], nck),
            ])
            for c in range(8):
                s = c % 4
                S.dma(f"Pw{s}", wst[s][:], w_in[c * 128:(c + 1) * 128, :], writes=[f"wst{s}"])
                cp(castrot.next(), win[:, c, :], wst[s][:], [f"wst{s}"], [f"win{c}"])
            WIN = [f"win{c}" for c in range(8)]
            cp("pool", winsw[:, :, 0:64], win[:, :, 1984:2048], WIN, ["winsw"])
            cp("pool", winsw[:, :, 64:80], win[:, :, 2064:2080], WIN, ["winsw"])
            cp("pool", winsw[:, :, 80:96], win[:, :, 2048:2064], WIN, ["winsw"])
            k = 0
            for (wd, wt, ncol, wn) in ((w_uq, wuq, 768, "wuq"), (w_uk, wuk, 512, "wuk"), (w_uv, wuv, 512, "wuv")):
                for c in range(2):
                    s = k % 4
                    k += 1
                    S.dma(f"Pw{s}", wst[s][:, 0:ncol], wd[c * 128:(c + 1) * 128, :], writes=[f"wst{s}"])
                    cp(castrot.next(), wt[:, c, :], wst[s][:, 0:ncol], [f"wst{s}"], [wn])
            wq4 = wuq[:].rearrange("p c (h f) -> p c h f", f=96)
            wqs4 = wuqsw[:].rearrange("p c (h f) -> p c h f", f=96)
            for c in range(2):
                cp("pool", wqs4[:, c, :, 0:64], wq4[:, c, :, 0:64], ["wuq"], ["wuqsw"])
                cp("pool", wqs4[:, c, :, 64:80], wq4[:, c, :, 80:96], ["wuq"], ["wuqsw"])
                cp("pool", wqs4[:, c, :, 80:96], wq4[:, c, :, 64:80], ["wuq"], ["wuqsw"])

            xin = [sb(f"P_xin{k}", [128, DM], F32) for k in range(3)]
            junk = sb("P_junk", [128, DM], BF16)
            xn = [sb(f"P_xn{k}", [128, DM], BF16) for k in range(2)]
            st1 = [sb(f"P_st{k}", [128, 4], F32) for k in range(3)]
            hT = [sb(f"P_hT{k}", [128, 8, 512], BF16) for k in range(2)]
            tabs = [sb(f"P_tabs{k}", [128, 4, 512], BF16) for k in range(2)]
            o32 = [sb(f"P_o32{k}", [128, 512], F32) for k in range(2)]
            pcp = [sb(f"P_pcp{k}", [128, 512], F32) for k in range(2)]
            tmp = [sb(f"P_tmp{k}", [128, 512], F32) for k in range(2)]
            qko = [sb(f"P_qko{k}", [128, 512], BF16) for k in range(3)]
            vst = [sb(f"P_vst{k}", [128, 768], BF16) for k in range(2)]
            vmst = [sb(f"P_vmst{k}", [128, 768], BF16) for k in range(2)]
            for k in range(2):
                memset("pool", vst[k][:], 1.0, [f"vst{k}"])
                memset("pool", vmst[k][:], 1.0, [f"vmst{k}"])
            sq = [sb(f"P_sq{k}", [128, 512], BF16) for k in range(2)]
            rstd = sb("P_rstd", [128, 512], F32)
            cqn = sb("P_cqn", [128, 2, 512], BF16)
            ckvn = sb("P_ckvn", [128, 2, 512], BF16)
            mqo = [sb(f"P_mqo{k}", [96, 512], BF16) for k in range(2)]
            mks = sb("P_mks", [96, 8, 512], BF16)
            kr = sb("P_kr", [96, 512], BF16)
            t1 = [sb(f"P_t1{k}", [96, 512], F32) for k in range(2)]
            t2 = [sb(f"P_t2{k}", [96, 512], F32) for k in range(2)]

            prot = Rot([2, 3, 4])
            nslot = Rot([0, 1])
            qslot = Rot([0, 1, 2])

            def vview(t):
                return t[:].rearrange("p (m x) -> p m x", x=192).rearrange("p m (a b) -> p m a b", b=64)[:, :, 0:3:2, :]

            def p_tiles(ck):
                hs = ck % 2
                tok0 = ck * 512
                S.dma(f"Ptab{hs}", tabs[hs][:], c_tabs[:, :, tok0:tok0 + 512].rearrange("f p t -> p f t"), writes=[f"tabs{hs}"])
                for tl in range(4):
                    ti = ck * 4 + tl
                    xs = ti % 3
                    ns = ti % 2
                    pb = ti % 2
                    S.dma(f"Px{xs}", xin[xs][:], x[ti * 128:(ti + 1) * 128, :], writes=[f"xin{xs}"])
                    S.op("act", lambda E, xs=xs: E.memzero(st1[xs][:, 0:1]), [], [f"ss{xs}"])
                    act(junk[:], xin[xs][:], AF.Square, [f"xin{xs}"], [f"junk{xs}", f"ss{xs}"], accum=st1[xs][:, 0:1])
                    act(st1[xs][:, 1:2], st1[xs][:, 0:1], AF.Sqrt, [f"ss{xs}"], [f"sd{xs}"], scale=1.0 / DM, bias=EPS)
                    recip(st1[xs][:, 2:3], st1[xs][:, 1:2], [f"sd{xs}"], [f"rs{xs}"])
                    act(xn[ns][:], xin[xs][:], AF.Copy, [f"xin{xs}", f"rs{xs}"], [f"xn{ns}"], scale=st1[xs][:, 2:3])
                    for c in range(8):
                        tr(psb(pb)[:, c * 128:(c + 1) * 128], xn[ns][:, c * 128:(c + 1) * 128], ident[:], [f"xn{ns}", "ident"], [f"ps{pb}"])
                    tt("dve", hT[hs][:, :, tl * 128:(tl + 1) * 128], psb(pb).rearrange("p (c t) -> p c t", t=128),
                       g1[:].unsqueeze(2).to_broadcast([128, 8, 128]), ALU.mult, [f"ps{pb}", "g1"], [f"hT{hs}_{tl}"])

            p_tiles(0)
            for ck in range(8):
                hs = ck % 2
                tok0 = ck * 512
                HK = [f"hT{hs}_{tl}" for tl in range(4)]
                tk = f"tabs{hs}"
                psec = (debug or {}).get("_psec", "qvlkmw")
                def emit_lat(li):
                    (lcol, gl, gname, dst, dkey) = ((1536, gq, "gq", cqn, "cqn"), (1792, gkv, "gkv", ckvn, "ckvn"))[li]
                    for blk in range(2):
                        b = 5 + blk
                        for c in range(8):
                            mm(psf(b), win[:, c, lcol + blk * 128:lcol + (blk + 1) * 128], hT[hs][:, c, :], c == 0, c == 7, [f"win{c}"] + HK, [f"ps{b}"])
                        act(sq[blk][:], psf(b), AF.Square, [f"ps{b}"], [f"sq{blk}"])
                    for blk in range(2):
                        mm(psf(7), ones[:], sq[blk][:], blk == 0, blk == 1, ["ones", f"sq{blk}"], ["ps7"])
                    act(rstd[:], psf(7), AF.Sqrt, ["ps7"], ["rstd"], scale=1.0 / 256.0, bias=EPS)
                    recip(rstd[:], rstd[:], ["rstd"], ["rstd"])
                    for blk in range(2):
                        stt("dve", dst[:, blk, :], psf(5 + blk), gl[:, blk:blk + 1], rstd[:], ALU.mult, ALU.mult,
                            [f"ps{5 + blk}", "rstd", gname], [f"{dkey}{blk}"])
                def emit_fb(fb):
                    col0 = fb * 128
                    b = prot.next()
                    for c in range(8):
                        mm(psf(b), win[:, c, col0:col0 + 128], hT[hs][:, c, :], c == 0, c == 7, [f"win{c}"] + HK, [f"ps{b}"])
                    s2 = nslot.next()
                    qs = qslot.next()
                    tt("dve", o32[s2][:], psf(b), tabs[hs][:, 0, :], ALU.mult, [f"ps{b}", tk], [f"o32{s2}"])
                    tt("dve", tmp[s2][0:32, :], psf(b)[32:64, :], tabs[hs][32:64, 1, :], ALU.mult, [f"ps{b}", tk], [f"tmp{s2}a"])
                    tt("dve", tmp[s2][32:64, :], psf(b)[0:32, :], tabs[hs][0:32, 1, :], ALU.mult, [f"ps{b}", tk], [f"tmp{s2}b"])
                    tt("dve", tmp[s2][64:96, :], psf(b)[96:128, :], tabs[hs][96:128, 1, :], ALU.mult, [f"ps{b}", tk], [f"tmp{s2}c"])
                    tt("dve", tmp[s2][96:128, :], psf(b)[64:96, :], tabs[hs][64:96, 1, :], ALU.mult, [f"ps{b}", tk], [f"tmp{s2}d"])
                    tt("pool", qko[qs][:], o32[s2][:], tmp[s2][:], ALU.add, [f"o32{s2}"] + [f"tmp{s2}{z}" for z in "abcd"], [f"qko{qs}"])
                    S.dma(f"Pqk{qs}", qkT[fb, :, tok0:tok0 + 512], qko[qs][:], reads=[f"qko{qs}"], writes=["qkT"])

                def emit_va(tl):
                    ti = ck * 4 + tl
                    b = prot.next()
                    vs = ti % 2
                    for c in range(8):
                        mm(psf(b), hT[hs][:, c, tl * 128:(tl + 1) * 128], win[:, c, 1024:1536], c == 0, c == 7, [f"win{c}", f"hT{hs}_{tl}"], [f"ps{b}"])
                    cp("act", vview(vst[vs]), psf(b).rearrange("p (m a b) -> p m a b", a=2, b=64), [f"ps{b}"], [f"vst{vs}"])
                    S.dma(f"Pva{vs}", vA[ti * 128:(ti + 1) * 128, :], vst[vs][:], reads=[f"vst{vs}"], writes=["vA"])

                for fb in range(8):
                    if "q" in psec:
                        emit_fb(fb)
                    if fb % 2 == 1 and "v" in psec:
                        emit_va(fb // 2)
                    if fb == 2 and "l" in psec:
                        emit_lat(0)
                    if fb == 5 and "l" in psec:
                        emit_lat(1)
                if ck + 1 < 8:
                    p_tiles(ck + 1)
                CQ = ["cqn0", "cqn1"]
                CKV = ["ckvn0", "ckvn1"]
                if "k" not in psec:
                    continue
                bA = prot.next()
                for c in range(8):
                    mm(psf(bA)[0:96, :], win[:, c, 1984:2080], hT[hs][:, c, :], c == 0, c == 7, [f"win{c}"] + HK, [f"ps{bA}"])
                bB = prot.next()
                for c in range(8):
                    mm(psf(bB)[0:96, :], winsw[:, c, :], hT[hs][:, c, :], c == 0, c == 7, ["winsw"] + HK, [f"ps{bB}"])
                tt("dve", t1[0][64:96, :], psf(bA)[64:96, :], tabs[hs][64:96, 2, :], ALU.mult, [f"ps{bA}", tk], ["t10"])
                tt("dve", t2[0][64:96, :], psf(bB)[64:96, :], tabs[hs][64:96, 3, :], ALU.mult, [f"ps{bB}", tk], ["t20"])
                tt("dve", kr[64:96, :], t1[0][64:96, :], t2[0][64:96, :], ALU.add, ["t10", "t20"], ["kr"])
                cp("dve", mks[64:96, :, :], kr[64:96, :].unsqueeze(1).to_broadcast([32, 8, 512]), ["kr"], ["mksr"])
                if "m" not in psec:
                    continue
                for h in range(8):
                    bA = prot.next()
                    for c in range(2):
                        mm(psf(bA)[0:96, :], wuq[:, c, h * 96:(h + 1) * 96], cqn[:, c, :], c == 0, c == 1, ["wuq"] + CQ, [f"ps{bA}"])
                    bB = prot.next()
                    for c in range(2):
                        mm(psf(bB)[0:96, :], wuqsw[:, c, h * 96:(h + 1) * 96], cqn[:, c, :], c == 0, c == 1, ["wuqsw"] + CQ, [f"ps{bB}"])
                    ms = h % 2
                    cp("act", mqo[ms][0:64, :], psf(bA)[0:64, :], [f"ps{bA}"], [f"mqo{ms}n"])
                    tt("dve", t1[ms][64:96, :], psf(bA)[64:96, :], tabs[hs][64:96, 2, :], ALU.mult, [f"ps{bA}", tk], [f"t1{ms}"])
                    tt("dve", t2[ms][64:96, :], psf(bB)[64:96, :], tabs[hs][64:96, 3, :], ALU.mult, [f"ps{bB}", tk], [f"t2{ms}"])
                    tt("dve", mqo[ms][64:96, :], t1[ms][64:96, :], t2[ms][64:96, :], ALU.add, [f"t1{ms}", f"t2{ms}"], [f"mqo{ms}r"])
                    S.dma(f"Pmq{ms}", mq[h, :, tok0:tok0 + 512], mqo[ms][:], reads=[f"mqo{ms}n", f"mqo{ms}r"], writes=["mq"])
                    bK = prot.next()
                    for c in range(2):
                        mm(psf(bK)[0:64, :], wuk[:, c, h * 64:(h + 1) * 64], ckvn[:, c, :], c == 0, c == 1, ["wuk"] + CKV, [f"ps{bK}"])
                    cp("act", mks[0:64, h, :], psf(bK)[0:64, :], [f"ps{bK}"], [f"mks{h}"])
                S.dma("Pmk", mk[:, :, tok0:tok0 + 512].rearrange("h p t -> p h t"), mks[:], reads=["mksr"] + [f"mks{h}" for h in range(8)], writes=["mk"])
                for tl in (range(4) if "w" in psec else []):
                    ti = ck * 4 + tl
                    b = prot.next()
                    vs = ti % 2
                    for c in range(2):
                        mm(psf(b), ckvn[:, c, tl * 128:(tl + 1) * 128], wuv[:, c, :], c == 0, c == 1, ["wuv"] + CKV, [f"ps{b}"])
                    cp("act", vview(vmst[vs]), psf(b).rearrange("p (m a b) -> p m a b", a=2, b=64), [f"ps{b}"], [f"vmst{vs}"])
                    S.dma(f"Pvm{vs}", vM[ti * 128:(ti + 1) * 128, :], vmst[vs][:], reads=[f"vmst{vs}"], writes=["vM"])

        def u_alloc(ph):
            sb = lambda n, s, d: ph.enter_context(nc.sbuf_tensor(n, s, d))
            NS = 3
            ub = {
                "ident": sb("U_ident", [128, 128], BF16),
                "ust": [sb(f"U_ust{k}", [128, DM], F32) for k in range(NS)],
                "vst": [sb(f"U_vst{k}", [128, DM], F32) for k in range(NS)],
                "ubf": [sb(f"U_ubf{k}", [128, DM], BF16) for k in range(NS)],
                "vbf": [sb(f"U_vbf{k}", [128, DM], BF16) for k in range(NS)],
                "utl": [sb(f"U_utl{k}", [128, 8 * 128], BF16) for k in range(NS)],
                "NS": NS,
            }
            S.dma("Uc", ub["ident"][:], c_ident, writes=["Uident"])
            return ub

        def u_gen(ub, bank=7):
            NS = ub["NS"]
            crotA = Rot(["act", "act", "act", "dve"])
            crotM = Rot(["dve", "pool"])

            class _C:
                def next(self_):
                    return (crotM if ub.get("inM") else crotA).next()
            crot = _C()

            def load(i):
                s = i % NS
                S.dma_group(f"Ul{s}", [
                    (ub["ust"][s][:], peer_u[i * 128:(i + 1) * 128, :], [], [f"Uust{s}"], {}),
                    (ub["vst"][s][:], peer_v[i * 128:(i + 1) * 128, :], [], [f"Uvst{s}"], {}),
                ], queue="act")
            load(0)
            load(1)
            for i in range(128):
                s = i % NS
                if i + 2 < 128:
                    load(i + 2)
                cp(crot.next(), ub["ubf"][s][:], ub["ust"][s][:], [f"Uust{s}"], [f"Uubf{s}"])
                cp(crot.next(), ub["vbf"][s][:], ub["vst"][s][:], [f"Uvst{s}"], [f"Uvbf{s}"])
                for c in range(8):
                    tr(psb(bank)[:, c * 128:(c + 1) * 128], ub["ubf"][s][:, c * 128:(c + 1) * 128], ub["ident"][:], [f"Uubf{s}", "Uident"], [f"ps{bank}"])
                cp("dve", ub["utl"][s][:], psb(bank), [f"ps{bank}"], [f"Uutl{s}"])
                S.dma_group(f"Us{s}", [
                    (UTd[i], ub["utl"][s][:], [f"Uutl{s}"], ["UTd"], {}),
                    (Vbd[i * 128:(i + 1) * 128, :], ub["vbf"][s][:], [f"Uvbf{s}"], ["Vbd"], {}),
                ])
                yield

        def phase_A(ph, ugen=None):
            sb = lambda n, s, d: ph.enter_context(nc.sbuf_tensor(n, s, d))
            mask2 = sb("A_mask2", [128, 256], BF16)
            S.dma("c0", mask2[:], c_mask2, writes=["mask2"])
            QT = [sb(f"A_QT{k}", [128, SEQ], BF16) for k in range(2)]
            KT = [sb(f"A_KT{k}", [128, SEQ], BF16) for k in range(2)]
            Vp = [sb(f"A_Vp{k}", [128, 32, 192], BF16) for k in range(3)]
            QTp = [sb(f"A_QTp{k}", [128, SEQ], BF16) for k in range(2)]
            KTp = [sb(f"A_KTp{k}", [128, SEQ], BF16) for k in range(2)]
            acc = [sb(f"A_acc{k}", [128, SEQ], F32) for k in range(2)]
            NPT = 8
            PT = [sb(f"A_PT{k}", [128, 256], BF16) for k in range(NPT)]
            rden = [sb(f"A_rden{k}", [128, 1024], F32) for k in range(2)]
            mixs = [sb(f"A_mixs{k}", [128, SEQ], BF16) for k in range(2)]
            srot = Rot([0, 1, 2, 3])
            prot = Rot([4, 5, 6])
            ptrot = Rot(list(range(NPT)))
            mrot = Rot(["pool", "dve"])
            D = 5
            PATS = (1, 4, 16)

            def load_qk(m):
                s = m % 2
                S.dma_group(f"Aq{s}", [
                    (QT[s][:, hf * 2048:(hf + 1) * 2048], qkT[m, :, hf * 2048:(hf + 1) * 2048], ["qkT"], [f"QT{s}"], {}) for hf in range(2)
                ] + [
                    (KT[s][:, hf * 2048:(hf + 1) * 2048], qkT[4 + m, :, hf * 2048:(hf + 1) * 2048], ["qkT"], [f"KT{s}"], {}) for hf in range(2)
                ])

            def load_vp(m, pi):
                d = PATS[pi]
                nb = 32 // d
                vs = pi
                vsrc = vA.rearrange("(n i r) c -> i r n c", i=128, r=d)
                items = []
                for r in range(d):
                    step = 8 if nb > 8 else nb
                    for n0 in range(0, nb, step):
                        items.append((Vp[vs][:, r * nb + n0:r * nb + n0 + step, :], vsrc[:, r, n0:n0 + step, 192 * m:192 * m + 192],
                                      ["vA"], [f"Vp{vs}"], {}))
                S.dma_group(f"Av{vs}", items)

            def prep_perm(m, pi):
                if pi == 0:
                    return
                d = PATS[pi]
                s = m % 2
                slot = pi - 1
                for hf in range(2):
                    r0, r1 = hf * d // 2, (hf + 1) * d // 2
                    srcq = QT[s][:, :].rearrange("p (m r) -> p r m", r=d)[:, r0:r1, :]
                    dstq = QTp[slot][:, :].rearrange("p (r m) -> p r m", r=d)[:, r0:r1, :]
                    srck = KT[s][:, :].rearrange("p (m r) -> p r m", r=d)[:, r0:r1, :]
                    dstk = KTp[slot][:, :].rearrange("p (r m) -> p r m", r=d)[:, r0:r1, :]
                    cp("act", dstq, srcq, [f"QT{s}"], [f"QTp{slot}_{hf}"])
                    cp("dve", dstk, srck, [f"KT{s}"], [f"KTp{slot}_{hf}"])

            its = []
            for m in range(4):
                for hh in range(2):
                    for pi, d in enumerate(PATS):
                        nb = 32 // d
                        pidx = 0
                        for r in range(d):
                            for n in range(nb):
                                pidx += 1
                                its.append(dict(m=m, pi=pi, d=d, nb=nb, hh=hh, r=r, n=n, k=pi,
                                                pat_first=(pidx == D + 1),
                                                head_last=(pi == 2 and r == d - 1 and n == nb - 1)))
            load_qk(0)
            for pi_ in range(3):
                load_vp(0, pi_)
            state = {"pvb": None}

            def stage1(it):
                m, d, r, n, hh, k = it["m"], it["d"], it["r"], it["n"], it["hh"], it["k"]
                s = m % 2
                if it["pat_first"]:
                    pi_ = it["pi"]
                    if hh == 0 and pi_ == 0:
                        prep_perm(m, 1)
                        prep_perm(m, 2)
                        if m > 0:
                            load_vp(m, 2)
                    if hh == 1 and pi_ == 0 and m + 1 < 4:
                        load_qk(m + 1)
                    if hh == 1 and pi_ >= 1 and m + 1 < 4:
                        load_vp(m + 1, pi_ - 1)
                rows = slice(hh * 64, (hh + 1) * 64)
                nb_ = it["nb"]
                if d == 1:
                    qsrc, ksrc = QT[s], KT[s]
                    rk = [f"KT{s}", f"QT{s}"]
                else:
                    slot = it["pi"] - 1
                    qsrc, ksrc = QTp[slot], KTp[slot]
                    rk = [f"KTp{slot}_0", f"KTp{slot}_1", f"QTp{slot}_0", f"QTp{slot}_1"]
                blk = r * nb_ + n
                qap = qsrc[rows, blk * 128:(blk + 1) * 128]
                sbk = srot.next()
                sps = psf(sbk)[:, 0:256]
                skey = f"ps{sbk}"
                pt = ptrot.next()
                it["pt"] = pt
                c0 = 0 if n > 0 else 128
                if n > 0:
                    mm(sps[:, 0:128], ksrc[rows, (blk - 1) * 128:blk * 128], qap, True, True, rk, [skey])
                mm(sps[:, 128:256], ksrc[rows, blk * 128:(blk + 1) * 128], qap, True, True, rk, [skey])
                act(PT[pt][:, c0:256], sps[:, c0:256], AF.Exp, [skey], [f"PT{pt}"], scale=0.125)
                tt("pool", PT[pt][:, c0:256], PT[pt][:, c0:256], mask2[:, c0:256], ALU.mult, [f"PT{pt}", "mask2"], [f"PT{pt}"])

            def stage2(it):
                m, d, r, n, hh, k, pi, nb = it["m"], it["d"], it["r"], it["n"], it["hh"], it["k"], it["pi"], it["nb"]
                s = m % 2
                vs = pi
                pt = it["pt"]
                grp = 4 if d < 16 else 2
                j = n % grp
                if j == 0:
                    state["pvb"] = prot.next()
                pvb = state["pvb"]
                vcols = slice(0, 128) if hh == 0 else slice(64, 192)
                pvo = psf(pvb)[:, j * 128:(j + 1) * 128]
                if n > 0:
                    mm(pvo, Vp[vs][:, r * nb + n - 1, vcols], PT[pt][:, 0:128], True, False, [f"Vp{vs}", f"PT{pt}"], [f"ps{pvb}"])
                mm(pvo, Vp[vs][:, r * nb + n, vcols], PT[pt][:, 128:256], n == 0, True, [f"Vp{vs}", f"PT{pt}"], [f"ps{pvb}"])
                if j == grp - 1:
                    n_first = n - (grp - 1)
                    t0 = n_first * 128 * d + r
                    span = grp * 128 * d
                    if d == 1:
                        ak = [f"acc{hh}_{t0 // 512}"]
                    elif d == 4:
                        ak = [f"acc{hh}_{(t0 // 512) + z}" for z in range(4)]
                    else:
                        ak = [f"acc{hh}_{z}" for z in range(8)]
                    dst = acc[hh][:, t0:t0 + span - d + 1:d]
                    src = psf(pvb)[:, 0:grp * 128]
                    if pi == 0:
                        cp("dve", dst, src, [f"ps{pvb}"], ak)
                    else:
                        tt("dve", dst, dst, src, ALU.add, [f"ps{pvb}"] + ak, ak)
                if it["head_last"]:
                    h2_ = hh
                    for cc in range(4):
                        cs = slice(cc * 1024, (cc + 1) * 1024)
                        ak = [f"acc{h2_}_{2 * cc}", f"acc{h2_}_{2 * cc + 1}"]
                        rs = cc % 2
                        if h2_ == 0:
                            recip(rden[rs][0:64, :], acc[0][64:128, cs], ak, [f"rden{rs}"])
                            tt("pool", mixs[s][0:64, cs], acc[0][0:64, cs], rden[rs][0:64, :], ALU.mult, ak + [f"rden{rs}"], [f"mixs{s}_{h2_}{cc}"])
                        else:
                            recip(rden[rs][64:128, :], acc[1][0:64, cs], ak, [f"rden{rs}"])
                            tt("dve", mixs[s][64:128, cs], acc[1][64:128, cs], rden[rs][64:128, :], ALU.mult, ak + [f"rden{rs}"], [f"mixs{s}_{h2_}{cc}"])
                    if hh == 1:
                        S.dma(f"Amx{s}", mixT[m], mixs[s][:], reads=[f"mixs{s}_{a_}{cc}" for a_ in range(2) for cc in range(4)], writes=["mixT"])

            NI = len(its)
            for idx in range(NI + D):
                if idx < NI:
                    stage1(its[idx])
                if idx >= D:
                    stage2(its[idx - D])
                if ugen is not None and idx % 12 == 11:
                    next(ugen, None)

        def phase_M(ph, ugen=None):
            sb = lambda n, s, d: ph.enter_context(nc.sbuf_tensor(n, s, d))
            if ugen is not None:
                UB["inM"] = True
            mask2 = sb("M_mask2", [128, 256], BF16)
            S.dma("c0", mask2[:], c_mask2, writes=["mask2"])
            QM = [sb(f"M_QM{k}", [96, SEQ], BF16) for k in range(2)]
            KM = [sb(f"M_KM{k}", [96, SEQ], BF16) for k in range(2)]
            VM = [sb(f"M_VM{k}", [128, 32, 128], BF16) for k in range(2)]
            NPT = 8
            PT = [sb(f"M_PT{k}", [128, 512], BF16) for k in range(NPT)]
            rden = [sb(f"M_rden{k}", [128, 512], F32) for k in range(2)]
            mixs = [sb(f"M_mixs{k}", [128, SEQ], BF16) for k in range(2)]
            srot = Rot([0, 1, 2, 3])
            arot = Rot([4, 5, 6])
            ptrot = Rot(list(range(NPT)))
            scale = 96.0 ** -0.5
            vsrc = vM.rearrange("(n i) c -> i n c", i=128)
            D = 5

            def load_head(h):
                s = h % 2
                m = h // 2
                c0 = 192 * m if h % 2 == 0 else 192 * m + 64
                items = []
                for hf in range(2):
                    items.append((QM[s][:, hf * 2048:(hf + 1) * 2048], mq[h, :, hf * 2048:(hf + 1) * 2048], ["mq"], [f"QM{s}"], {}))
                    items.append((KM[s][:, hf * 2048:(hf + 1) * 2048], mk[h, :, hf * 2048:(hf + 1) * 2048], ["mk"], [f"KM{s}"], {}))
                for n0 in range(0, 32, 8):
                    items.append((VM[s][:, n0:n0 + 8, :], vsrc[:, n0:n0 + 8, c0:c0 + 128], ["vM"], [f"VM{s}"], {}))
                S.dma_group(f"Ml{s}", items)

            its = []
            for h in range(8):
                hidx = 0
                for g in range(8):
                    for j in range(4 * g + 4):
                        hidx += 1
                        its.append(dict(h=h, g=g, j=j, head_first=(hidx == D + 1)))
            load_head(0)
            state = {"ab": None}

            def stage1(it):
                h, g, j = it["h"], it["g"], it["j"]
                s = h % 2
                if it["head_first"] and h + 1 < 8:
                    load_head(h + 1)
                b0 = max(0, j - 4 * g)
                cols = slice(b0 * 128, 512)
                sbk = srot.next()
                pt = ptrot.next()
                it["pt"] = pt
                mm(psf(sbk)[:, cols], KM[s][:, j * 128:(j + 1) * 128], QM[s][:, g * 512 + b0 * 128:(g + 1) * 512], True, True,
                   [f"KM{s}", f"QM{s}"], [f"ps{sbk}"])
                act(PT[pt][:, cols], psf(sbk)[:, cols], AF.Exp, [f"ps{sbk}"], [f"PT{pt}"], scale=scale)
                if j >= 4 * g:
                    dc = slice(b0 * 128, (b0 + 1) * 128)
                    tt("pool", PT[pt][:, dc], PT[pt][:, dc], mask2[:, 128:256], ALU.mult, [f"PT{pt}", "mask2"], [f"PT{pt}"])

            def stage2(it):
                h, g, j = it["h"], it["g"], it["j"]
                s = h % 2
                m = h // 2
                ms = m % 2
                pt = it["pt"]
                if j == 0:
                    state["ab"] = arot.next()
                ab = state["ab"]
                b0 = max(0, j - 4 * g)
                cols = slice(b0 * 128, 512)
                mm(psf(ab)[:, cols], VM[s][:, j, :], PT[pt][:, cols], j == 0, j == 4 * g + 3, [f"VM{s}", f"PT{pt}"], [f"ps{ab}"])
                if j == 4 * g + 3:
                    rs = g % 2
                    gs = slice(g * 512, (g + 1) * 512)
                    mk_ = f"mixs{ms}_{h % 2}{g}"
                    if h % 2 == 0:
                        recip(rden[rs][0:64, :], psf(ab)[64:128, :], [f"ps{ab}"], [f"rden{rs}"])
                        tt("dve", mixs[ms][0:64, gs], psf(ab)[0:64, :], rden[rs][0:64, :], ALU.mult, [f"ps{ab}", f"rden{rs}"], [mk_])
                    else:
                        recip(rden[rs][64:128, :], psf(ab)[0:64, :], [f"ps{ab}"], [f"rden{rs}"])
                        tt("dve", mixs[ms][64:128, gs], psf(ab)[64:128, :], rden[rs][64:128, :], ALU.mult, [f"ps{ab}", f"rden{rs}"], [mk_])
                    if g == 7 and h % 2 == 1:
                        S.dma(f"Mmx{ms}", mixT[4 + m], mixs[ms][:], reads=[f"mixs{ms}_{a}{gg}" for a in range(2) for gg in range(8)], writes=["mixT"])

            NI = len(its)
            for idx in range(NI + D):
                if idx < NI:
                    stage1(its[idx])
                if idx >= D:
                    stage2(its[idx - D])
                if ugen is not None and idx % 18 == 17:
                    next(ugen, None)
            if ugen is not None:
                for _ in ugen:
                    pass

        def phase_X(ph):
            sb = lambda n, s, d: ph.enter_context(nc.sbuf_tensor(n, s, d))
            ident = sb("X_ident", [128, 128], BF16)
            wo = sb("X_wo", [128, 8, DM], BF16)
            wst = [sb(f"X_wst{k}", [128, DM], F32) for k in range(4)]
            g2 = sb("X_g2", [128, 8], F32)
            S.dma_group("c0", [
                (ident[:], c_ident, [], ["ident"], {}),
                (g2[:], g_ffn.rearrange("(c p) -> p c", p=128), [], ["g2"], {"allow_slow_non_contiguous": True}),
            ])
            for c in range(8):
                s = c % 4
                S.dma(f"Xw{s}", wst[s][:], w_o[c * 128:(c + 1) * 128, :], writes=[f"wst{s}"])
                cp(castrot.next(), wo[:, c, :], wst[s][:], [f"wst{s}"], [f"wo{c}"])
            mixin = [sb(f"X_mix{k}", [128, 8, 512], BF16) for k in range(2)]
            xin = [sb(f"X_xin{k}", [128, DM], F32) for k in range(3)]
            x1 = [sb(f"X_x1{k}", [128, DM], F32) for k in range(3)]
            junk = sb("X_junk", [128, DM], BF16)
            xn = [sb(f"X_xn{k}", [128, DM], BF16) for k in range(3)]
            st1 = [sb(f"X_st{k}", [128, 4], F32) for k in range(3)]
            h2 = [sb(f"X_h2{k}", [128, 8, 128], BF16) for k in range(2)]
            prot = Rot([(2, 3), (4, 5), (6, 7)])
            def load_mix(ck):
                mslot = ck % 2
                S.dma(f"Xm{mslot}", mixin[mslot][:], mixT[:, :, ck * 512:(ck + 1) * 512].rearrange("c p t -> p c t"), reads=["mixT"], writes=[f"mixin{mslot}"])

            load_mix(0)

            def x_partA(ti):
                    ck, tl = divmod(ti, 4)
                    mslot = ck % 2
                    if tl == 0 and ck + 1 < 8:
                        load_mix(ck + 1)
                    xs = ti % 3
                    ns = ti % 3
                    S.dma(f"Xx{xs}", xin[xs][:], x[ti * 128:(ti + 1) * 128, :], writes=[f"xin{xs}"])
                    ba, bb = prot.next()
                    for hf, b in ((0, ba), (1, bb)):
                        for c in range(8):
                            mm(psf(b), mixin[mslot][:, c, tl * 128:(tl + 1) * 128], wo[:, c, hf * 512:(hf + 1) * 512], c == 0, c == 7,
                               [f"mixin{mslot}", f"wo{c}"], [f"ps{b}"])
                        tt("dve", x1[xs][:, hf * 512:(hf + 1) * 512], psf(b), xin[xs][:, hf * 512:(hf + 1) * 512], ALU.add,
                           [f"ps{b}", f"xin{xs}"], [f"x1{xs}_{hf}"])
                    XK = [f"x1{xs}_0", f"x1{xs}_1"]
                    S.dma(f"Xo{xs}", x1d[ti * 128:(ti + 1) * 128, :], x1[xs][:], reads=XK, writes=["x1d"])
                    S.op("act", lambda E, xs=xs: E.memzero(st1[xs][:, 0:1]), [], [f"ss{xs}"])
                    act(junk[:], x1[xs][:], AF.Square, XK, [f"junk{xs}", f"ss{xs}"], accum=st1[xs][:, 0:1])
                    act(st1[xs][:, 1:2], st1[xs][:, 0:1], AF.Sqrt, [f"ss{xs}"], [f"sd{xs}"], scale=1.0 / DM, bias=EPS)
                    recip(st1[xs][:, 2:3], st1[xs][:, 1:2], [f"sd{xs}"], [f"rs{xs}"])
                    act(xn[ns][:], x1[xs][:], AF.Copy, XK + [f"rs{xs}"], [f"xn{ns}"], scale=st1[xs][:, 2:3])

            def x_partB(ti):
                    ns = ti % 3
                    hs_ = ti % 2
                    pb = ti % 2
                    for c in range(8):
                        tr(psb(pb)[:, c * 128:(c + 1) * 128], xn[ns][:, c * 128:(c + 1) * 128], ident[:], [f"xn{ns}", "ident"], [f"ps{pb}"])
                    tt("dve", h2[hs_][:], psb(pb).rearrange("p (c t) -> p c t", t=128),
                       g2[:].unsqueeze(2).to_broadcast([128, 8, 128]), ALU.mult, [f"ps{pb}", "g2"], [f"h2{hs_}"])
                    S.dma(f"Xh{hs_}", h2Td[:, :, ti * 128:(ti + 1) * 128], h2[hs_][:], reads=[f"h2{hs_}"], writes=["h2Td"])

            x_partA(0)
            x_partA(1)
            for ti in range(NT):
                if ti + 2 < NT:
                    x_partA(ti + 2)
                x_partB(ti)

        def phase_O(ph):
            sb = lambda n, s, d: ph.enter_context(nc.sbuf_tensor(n, s, d))
            ident = sb("O_ident", [128, 128], BF16)
            identf = sb("O_identf", [128, 128], F32)
            iota = sb("O_iota", [128, 128], BF16)
            iota16 = sb("O_iota16", [128, 16], F32)
            S.dma_group("c0", [
                (ident[:], c_ident, [], ["ident"], {}),
                (identf[:], c_identf, [], ["identf"], {}),
                (iota[:], c_iota, [], ["iota"], {}),
                (iota16[:], c_iota16, [], ["iota16"], {}),
            ])
            wq = sb("O_wq", [128, 8, 2048], BF16)
            wst = [sb(f"O_wst{k}", [128, 1024], F32) for k in range(2)]
            eq = [sb(f"O_eq{k}", [128, 8, 16, 16], F32) for k in range(2)]
            wv = [wst[0][:], wst[1][:]]
            for k_ in range(2):
                fl = eq[k_][:].rearrange("p a b c -> p (a b c)")
                wv += [fl[:, 0:1024], fl[:, 1024:2048]]
            NWS = len(wv)
            wi = 0
            for c in range(8):
                for hf in range(2):
                    s = wi % NWS
                    wi += 1
                    S.dma(f"Ow{s}", wv[s], w_query[c * 128:(c + 1) * 128, hf * 1024:(hf + 1) * 1024], writes=[f"wst{s}"])
                    cp(castrot.next(), wq[:, c, hf * 1024:(hf + 1) * 1024], wv[s], [f"wst{s}"], [f"wq{c}"])
            skb = sb("O_skb", [128, 16, 128], BF16)
            skT = sb("O_skT", [128, 16, 128], BF16)
            for hp in range(16):
                s = wi % NWS
                wi += 1
                S.dma(f"Ow{s}", wv[s][:, 0:128], sub_keys[hp * 128:(hp + 1) * 128, :], writes=[f"wst{s}"])
                cp("dve", skb[:, hp, :], wv[s][:, 0:128], [f"wst{s}"], ["skb"])
            for half in range(2):
                for k in range(8):
                    hp = half * 8 + k
                    tr(psb(half)[:, k * 128:(k + 1) * 128], skb[:, hp, :], ident[:], ["skb", "ident"], [f"ps{half}"])
                cp("dve", skT[:, half * 8:(half + 1) * 8, :], psb(half).rearrange("p (k n) -> p k n", n=128), [f"ps{half}"], ["skT"])

            TB = 8
            h2t = [sb(f"O_h2t{k}", [128, 8, 128], BF16) for k in range(2)]
            qT = [sb(f"O_qT{k}", [128, 16, 128], BF16) for k in range(2)]
            sc = [sb(f"O_sc{k}", [128, 16, 128], F32) for k in range(2)]
            scw = sb("O_scw", [128, 16, 128], F32)
            top = sb("O_top", [128, 16, 16], F32)
            topi = sb("O_topi", [128, 16, 16], U32)
            topf = sb("O_topf", [128, 16, 16], F32)
            cand = sb("O_cand", [128, 8, 256], F32)
            candw = sb("O_candw", [128, 8, 256], F32)
            best = sb("O_best", [128, 8, 16], F32)
            posi = sb("O_posi", [128, 8, 16], U32)
            k1u = sb("O_k1u", [128, 8, 16], U32)
            posf = sb("O_posf", [128, 8, 16], F32)
            k0f = sb("O_k0f", [128, 8, 16], F32)
            k1f = sb("O_k1f", [128, 8, 16], F32)
            i0f = sb("O_i0f", [128, 128], F32)
            i1f = sb("O_i1f", [128, 128], F32)
            gat = sb("O_gat", [128, 128], F32)
            ssum = sb("O_ssum", [128, 8], F32)
            pkT = [sb(f"O_pkT{k}", [128, 3, 128], BF16) for k in range(2)]
            pk32 = [sb(f"O_pk32{k}", [128, 2, 128], F32) for k in range(2)]
            NOH = 3
            oh1 = [sb(f"O_oh1{k}", [128, TB, 128], BF16) for k in range(NOH)]
            oh0g = [sb(f"O_oh0g{k}", [128, TB, 128], BF16) for k in range(NOH)]
            Gsb = [sb(f"O_G{k}", [128, 128, 128], BF16) for k in range(2)]
            grot = Rot([5, 6, 7])
            TRB = 4

            def stage_A(ti):
                hs = ti % 2
                S.dma(f"Oh{hs}", h2t[hs][:], h2Td[:, :, ti * 128:(ti + 1) * 128], reads=["h2Td"], writes=[f"h2t{hs}"])
                for q4 in range(4):
                    bk = q4 % 2
                    for k in range(4):
                        hp = q4 * 4 + k
                        o = psf(bk)[:, k * 128:(k + 1) * 128]
                        for c in range(8):
                            mm(o, wq[:, c, hp * 128:(hp + 1) * 128], h2t[hs][:, c, :], c == 0, c == 7, [f"wq{c}", f"h2t{hs}"], [f"ps{bk}"])
                        if k % 2 == 1:
                            yield
                    cp("act", qT[hs][:, q4 * 4:(q4 + 1) * 4, :], psf(bk).rearrange("p (k t) -> p k t", t=128), [f"ps{bk}"], [f"qT{hs}_{q4}"])
                for q4 in range(4):
                    bk = 2 + q4 % 2
                    for k in range(4):
                        hp = q4 * 4 + k
                        mm(psf(bk)[:, k * 128:(k + 1) * 128], qT[hs][:, hp, :], skT[:, hp, :], True, True, [f"qT{hs}_{q4}", "skT"], [f"ps{bk}"])
                    cp("act", sc[hs][:, q4 * 4:(q4 + 1) * 4, :], psf(bk).rearrange("p (k n) -> p k n", n=128), [f"ps{bk}"], [f"sc{hs}_{q4}"])
                    yield

            def stage_B(ti):
                hs = ti % 2
                for hp in range(16):
                    S.op("dve", lambda E, hp=hp, hs=hs: E.max(out=top[:, hp, 0:8], in_=sc[hs][:, hp, :]), [f"sc{hs}_{hp // 4}"], [f"tA{hp}"])
                yield
                for hp in range(16):
                    S.op("dve", lambda E, hp=hp, hs=hs: E.max_index(out=topi[:, hp, 0:8], in_max=top[:, hp, 0:8], in_values=sc[hs][:, hp, :]),
                         [f"sc{hs}_{hp // 4}", f"tA{hp}"], [f"tiA{hp}"])
                    S.op("dve", lambda E, hp=hp, hs=hs: E.match_replace(out=scw[:, hp, :], in_to_replace=top[:, hp, 0:8], in_values=sc[hs][:, hp, :], imm_value=-1e30),
                         [f"sc{hs}_{hp // 4}", f"tA{hp}"], [f"scw{hp}"])
                    if hp % 8 == 7:
                        yield
                for hp in range(16):
                    S.op("dve", lambda E, hp=hp: E.max(out=top[:, hp, 8:16], in_=scw[:, hp, :]), [f"scw{hp}"], [f"tB{hp}"])
                yield
                for hp in range(16):
                    S.op("dve", lambda E, hp=hp: E.max_index(out=topi[:, hp, 8:16], in_max=top[:, hp, 8:16], in_values=scw[:, hp, :]),
                         [f"scw{hp}", f"tB{hp}"], [f"tiB{hp}"])
                TOPK = [f"tA{hp}" for hp in range(16)] + [f"tB{hp}" for hp in range(16)]
                TOPI = [f"tiA{hp}" for hp in range(16)] + [f"tiB{hp}" for hp in range(16)]
                top5 = top[:].rearrange("p (h two) k -> p h two k", two=2)
                topf5 = topf[:].rearrange("p (h two) k -> p h two k", two=2)
                tt("dve", cand[:].rearrange("p h (a b) -> p h a b", b=16),
                   top5[:, :, 0, :].unsqueeze(3).to_broadcast([128, 8, 16, 16]),
                   top5[:, :, 1, :].unsqueeze(2).to_broadcast([128, 8, 16, 16]), ALU.add, TOPK, ["cand"])
                cp("dve", topf[:], topi[:], TOPI, ["topf"])
                yield
                for h in range(8):
                    S.op("dve", lambda E, h=h: E.max(out=best[:, h, 0:8], in_=cand[:, h, :]), ["cand"], [f"bA{h}"])
                yield
                for h in range(8):
                    S.op("dve", lambda E, h=h: E.max_index(out=posi[:, h, 0:8], in_max=best[:, h, 0:8], in_values=cand[:, h, :]), ["cand", f"bA{h}"], [f"pA{h}"])
                    S.op("dve", lambda E, h=h: E.match_replace(out=candw[:, h, :], in_to_replace=best[:, h, 0:8], in_values=cand[:, h, :], imm_value=-1e30),
                         ["cand", f"bA{h}"], [f"cw{h}"])
                yield
                for h in range(8):
                    S.op("dve", lambda E, h=h: E.max(out=best[:, h, 8:16], in_=candw[:, h, :]), [f"cw{h}"], [f"bB{h}"])
                yield
                for h in range(8):
                    S.op("dve", lambda E, h=h: E.max_index(out=posi[:, h, 8:16], in_max=best[:, h, 8:16], in_values=candw[:, h, :]), [f"cw{h}", f"bB{h}"], [f"pB{h}"])
                BEST = [f"bA{h}" for h in range(8)] + [f"bB{h}" for h in range(8)]
                POS = [f"pA{h}" for h in range(8)] + [f"pB{h}" for h in range(8)]
                gv = gat[:].rearrange("p (h k) -> p h k", k=16)
                tt("dve", gv, best[:], best[:, :, 0:1].to_broadcast([128, 8, 16]), ALU.subtract, BEST, ["gat"])
                act(gat[:], gat[:], AF.Exp, ["gat"], ["gat"])
                S.op("dve", lambda E: E.tensor_single_scalar(out=k1u[:], in_=posi[:], scalar=15, op=ALU.bitwise_and), POS, ["k1u"])
                cp("dve", posf[:], posi[:], POS, ["posf"])
                yield
                cp("dve", k1f[:], k1u[:], ["k1u"], ["k1f"])
                tt("dve", k0f[:], posf[:], k1f[:], ALU.subtract, ["posf", "k1f"], ["k0f"])
                ts("dve", k0f[:], k0f[:], 1.0 / 16.0, None, ALU.mult, None, ["k0f"], ["k0f"])
                S.op("dve", lambda E: E.tensor_reduce(out=ssum[:], in_=gat[:].rearrange("p (h k) -> p h k", k=16), axis=AX.X, op=ALU.add), ["gat"], ["ssum"])
                recip(ssum[:], ssum[:], ["ssum"], ["ssum"])
                yield
                io4 = iota16[:].unsqueeze(1).unsqueeze(1).to_broadcast([128, 8, 16, 16])
                i0v = i0f[:].rearrange("p (h k) -> p h k", k=16)
                i1v = i1f[:].rearrange("p (h k) -> p h k", k=16)
                tt("dve", eq[1][:], k1f[:].unsqueeze(3).to_broadcast([128, 8, 16, 16]), io4, ALU.is_equal, ["k1f", "iota16"], ["eq1"])
                tt("dve", eq[1][:], eq[1][:], topf5[:, :, 1, :].unsqueeze(2).to_broadcast([128, 8, 16, 16]), ALU.mult, ["eq1", "topf"], ["eq1"])
                yield
                tt("dve", eq[0][:], k0f[:].unsqueeze(3).to_broadcast([128, 8, 16, 16]), io4, ALU.is_equal, ["k0f", "iota16"], ["eq0"])
                tt("dve", eq[0][:], eq[0][:], topf5[:, :, 0, :].unsqueeze(2).to_broadcast([128, 8, 16, 16]), ALU.mult, ["eq0", "topf"], ["eq0"])
                tt("dve", gv, gv, ssum[:].unsqueeze(2).to_broadcast([128, 8, 16]), ALU.mult, ["gat", "ssum"], ["gat"])
                yield
                S.op("dve", lambda E: E.tensor_reduce(out=i1v, in_=eq[1][:], axis=AX.X, op=ALU.add), ["eq1"], ["i1f"])
                yield
                S.op("dve", lambda E: E.tensor_reduce(out=i0v, in_=eq[0][:], axis=AX.X, op=ALU.add), ["eq0"], ["i0f"])
                for k, (src, sname) in enumerate(((i0f, "i0f"), (i1f, "i1f"), (gat, "gat"))):
                    S.op("pe", lambda E, k=k, src=src: E.transpose(psf(TRB)[:, k * 128:(k + 1) * 128], src[:], identf[:]), [sname, "identf"], [f"ps{TRB}"])
                cp("act", pkT[hs][:], psf(TRB)[:, 0:384].rearrange("p (k t) -> p k t", t=128), [f"ps{TRB}"], [f"pkT{hs}"])
                cp("act", pk32[hs][:, 0, :], psf(TRB)[:, 0:128], [f"ps{TRB}"], [f"pk32{hs}"])
                cp("act", pk32[hs][:, 1, :], psf(TRB)[:, 256:384], [f"ps{TRB}"], [f"pk32{hs}"])
                yield

            ohc = {"n": 0}

            def stage_C(ti):
                hs = ti % 2
                gs = ti % 2
                pk = f"pkT{hs}"
                iob = iota[:].unsqueeze(1).to_broadcast([128, TB, 128])
                NB = 128 // TB

                for b in range(NB):
                    os_ = (ohc["n"] + b) % NOH
                    t0 = b * TB
                    tt("dve", oh1[os_][:], iob, pkT[hs][:, 1, t0:t0 + TB].unsqueeze(2).to_broadcast([128, TB, 128]), ALU.is_equal, ["iota", pk], [f"oh1{os_}"])
                    for tloc in range(TB):
                        ts("dve", oh0g[os_][:, tloc, :], iota[:], pk32[hs][:, 0, t0 + tloc:t0 + tloc + 1], pk32[hs][:, 1, t0 + tloc:t0 + tloc + 1],
                           ALU.is_equal, ALU.mult, ["iota", f"pk32{hs}"], [f"oh0g{os_}_{tloc}"])
                    for t4 in range(0, TB, 4):
                        gb = grot.next()
                        for tq in range(4):
                            tloc = t4 + tq
                            mm(psf(gb)[:, tq:509 + tq:4], oh1[os_][:, tloc, :], oh0g[os_][:, tloc, :], True, True,
                               [f"oh1{os_}", f"oh0g{os_}_{tloc}"], [f"ps{gb}"])
                        tg = t0 + t4
                        cp("act", Gsb[gs][:, :, tg:tg + 4], psf(gb).rearrange("j (i t) -> j i t", t=4), [f"ps{gb}"], [f"G{gs}_{tg}"])
                    yield
                ohc["n"] += NB
                GK = [f"G{gs}_{tg}" for tg in range(0, 128, 4)]
                for i0 in range(0, 128, 8):
                    S.dma(f"Og{gs}", Gd[i0:i0 + 8, :, ti * 128:(ti + 1) * 128].rearrange("i j t -> j i t"), Gsb[gs][:, i0:i0 + 8, :],
                          reads=GK, writes=["Gd"])
                yield

            for _ in stage_A(0):
                pass
            for t in range(NT + 1):
                gens = []
                if t < NT:
                    gB = stage_B(t)
                    gens += [gB, gB]
                if t >= 1:
                    gens.append(stage_C(t - 1))
                if t + 1 < NT:
                    gens.append(stage_A(t + 1))
                while gens:
                    for g_ in list(gens):
                        if g_ not in gens:
                            continue
                        try:
                            next(g_)
                        except StopIteration:
                            while g_ in gens:
                                gens.remove(g_)

        def phase_E(ph):
            sb = lambda n, s, d: ph.enter_context(nc.sbuf_tensor(n, s, d))
            TG = 1024
            NG = SEQ // TG
            SBK = 8
            NR = 16
            NSB = 128 // SBK
            NSG = TG // 256
            gfin = sb("E_gfin", [128, DM], F32)
            S.dma("c0", gfin[:], g_fin.partition_broadcast(128), writes=["gfin"])
            x1s = [sb(f"E_x1{k}", [128, TG // 128, DM], F32) for k in range(2)]
            h2Ts = [sb(f"E_h2T{k}", [128, 8, TG], BF16) for k in range(2)]
            UTs = [sb(f"E_UT{k}", [128, 8 * 128], BF16) for k in range(NR)]
            Vbs = [sb(f"E_Vb{k}", [128, DM], BF16) for k in range(NR)]
            Gts = [sb(f"E_Gt{k}", [128, TG], BF16) for k in range(NR)]
            NW = 4
            gel = [sb(f"E_gel{k}", [128, 256], BF16) for k in range(NW)]
            WT = [sb(f"E_WT{k}", [128, 256], BF16) for k in range(NW)]
            junk = sb("E_junk", [128, DM], BF16)
            st1 = [sb(f"E_st{k}", [128, 4], F32) for k in range(2)]
            ost = [sb(f"E_ost{k}", [128, DM], F32) for k in range(1)]
            arot = Rot([0, 1, 6, 7])
            grot = Rot(list(range(NW)))
            wrot = Rot(["dve", "pool"])
            D = 2

            SBL = [(g, sbk) for g in range(NG) for sbk in range(NSB)]

            def load_sb(k):
                g, sbk = SBL[k]
                tok0 = g * TG
                for ii in range(SBK):
                    i = sbk * SBK + ii
                    r = (k * SBK + ii) % NR
                    S.dma_group(f"El{r}", [
                        (UTs[r][:], UTd[i], ["UTd"], [f"UT{r}"], {}),
                        (Vbs[r][:], Vbd[i * 128:(i + 1) * 128, :], ["Vbd"], [f"Vb{r}"], {}),
                        (Gts[r][:], Gd[i, :, tok0:tok0 + TG], ["Gd"], [f"Gt{r}"], {}),
                    ])

            def load_group(g):
                tok0 = g * TG
                gb = g % 2
                S.dma_group(f"Ex1{gb}", [
                    (x1s[gb][:, tl, :], x1d[tok0 + tl * 128:tok0 + (tl + 1) * 128, :], ["x1d"], [f"x1{gb}_{tl}_0", f"x1{gb}_{tl}_1"], {}) for tl in range(TG // 128)
                ] + [(h2Ts[gb][:], h2Td[:, :, tok0:tok0 + TG], ["h2Td"], [f"h2T{gb}"], {})])

            its = []
            for k, (g, sbk) in enumerate(SBL):
                for sg in range(NSG):
                    for ii in range(SBK):
                        its.append(dict(k=k, g=g, sbk=sbk, sg=sg, ii=ii,
                                        sb_first=(sg == 0 and ii == D), grp_first=(sbk == 0 and sg == 0 and ii == 0),
                                        grp_last=(sbk == NSB - 1 and sg == NSG - 1 and ii == SBK - 1)))
            S.dma_group("Ex10", [(h2Ts[0][:], h2Td[:, :, 0:TG], ["h2Td"], ["h2T0"], {})])
            load_sb(0)
            S.dma_group("Ex10", [
                (x1s[0][:, tl, :], x1d[tl * 128:(tl + 1) * 128, :], ["x1d"], [f"x10_{tl}_0", f"x10_{tl}_1"], {}) for tl in range(TG // 128)
            ])

            def stage1(it):
                k, g, sg, ii = it["k"], it["g"], it["sg"], it["ii"]
                if it["sb_first"] and it["sbk"] == 0 and g + 1 < NG:
                    load_group(g + 1)
                if it["sb_first"] and k + 1 < len(SBL):
                    load_sb(k + 1)
                x1 = x1s[g % 2]
                h2T = h2Ts[g % 2]
                gb = g % 2
                r = (k * SBK + ii) % NR
                ab = arot.next()
                akey = f"ps{ab}"
                ao = psf(ab)[:, 0:256]
                for c in range(8):
                    mm(ao, UTs[r][:, c * 128:(c + 1) * 128], h2T[:, c, sg * 256:(sg + 1) * 256], c == 0, c == 7, [f"UT{r}", f"h2T{gb}"], [akey])
                gsl = grot.next()
                it["gsl"] = gsl
                act(gel[gsl][:], ao, AF.Gelu, [akey], [f"gel{gsl}"])
                tt(wrot.next(), WT[gsl][:], gel[gsl][:], Gts[r][:, sg * 256:(sg + 1) * 256], ALU.mult, [f"gel{gsl}", f"Gt{r}"], [f"WT{gsl}"])

            def stage2(it):
                k, g, sg, ii = it["k"], it["g"], it["sg"], it["ii"]
                r = (k * SBK + ii) % NR
                gsl = it["gsl"]
                x1 = x1s[g % 2]
                gb = g % 2
                for tl in range(2):
                    for hf in range(2):
                        b = 2 + tl * 2 + hf
                        mm(psf(b), WT[gsl][:, tl * 128:(tl + 1) * 128], Vbs[r][:, hf * 512:(hf + 1) * 512], ii == 0, ii == SBK - 1,
                           [f"WT{gsl}", f"Vb{r}"], [f"ps{b}"])
                if ii == SBK - 1:
                    for tl in range(2):
                        tix = sg * 2 + tl
                        for hf in range(2):
                            b = 2 + tl * 2 + hf
                            xk = f"x1{gb}_{tix}_{hf}"
                            tt("dve", x1[:, tix, hf * 512:(hf + 1) * 512], x1[:, tix, hf * 512:(hf + 1) * 512], psf(b), ALU.add, [xk, f"ps{b}"], [xk])
                if it["grp_last"]:
                    tok0 = g * TG
                    for tl in range(TG // 128):
                        s = tl % 2
                        XK = [f"x1{gb}_{tl}_0", f"x1{gb}_{tl}_1"]
                        S.op("act", lambda E, s=s: E.memzero(st1[s][:, 0:1]), [], [f"ss{s}"])
                        act(junk[:], x1[:, tl, :], AF.Square, XK, [f"junk{s}", f"ss{s}"], accum=st1[s][:, 0:1])
                        act(st1[s][:, 1:2], st1[s][:, 0:1], AF.Sqrt, [f"ss{s}"], [f"sd{s}"], scale=1.0 / DM, bias=EPS)
                        recip(st1[s][:, 2:3], st1[s][:, 1:2], [f"sd{s}"], [f"rs{s}"])
                        stt("dve", ost[0][:], x1[:, tl, :], st1[s][:, 2:3], gfin[:], ALU.mult, ALU.mult, XK + [f"rs{s}", "gfin"], ["ost0"])
                        S.dma("Eo0", out[tok0 + tl * 128:tok0 + (tl + 1) * 128, :], ost[0][:], reads=["ost0"], writes=["out"])

            NI = len(its)
            for idx in range(NI + D):
                if idx < NI:
                    stage1(its[idx])
                if idx >= D and not its[idx - D].get("done"):
                    stage2(its[idx - D])
                    its[idx - D]["done"] = True

        phases = [("P", phase_P), ("A", phase_A), ("M", phase_M), ("X", phase_X), ("O", phase_O), ("E", phase_E)]
        only = (debug or {}).get("_only")
        ustack = ExitStack()
        ugen = None
        UB = {}
        for name, fn in phases:
            if only is None or name in only:
                if name == "A" and (only is None or "U" in only):
                    UB.update(u_alloc(ustack))
                    ugen = u_gen(UB)
                with ExitStack() as ph:
                    if name in ("A", "M"):
                        fn(ph, ugen)
                    else:
                        fn(ph)
                    S.barrier()
                if name == "M":
                    ustack.close()
            if name == stop_after:
                break
        ustack.close()
        S.barrier()
        with nc.Block() as block:
            S.replay(block)
        build_program.stats = (S.n_ins, S.n_wait, dict(S.cnt))
    return nc


_CONSTS = None


def _layout_inputs(inputs):
    global _CONSTS
    if _CONSTS is None:
        _CONSTS = _host_consts()
    f = lambda a: np.ascontiguousarray(np.asarray(a, dtype=np.float32))
    shared = {
        "attn_norm_g": f(inputs["attn_norm_g"]).reshape(DM),
        "w_in": f(inputs["w_in"]).reshape(DM, 2080),
        "mla_q_norm_g": f(inputs["mla_q_norm_g"]).reshape(256),
        "mla_kv_norm_g": f(inputs["mla_kv_norm_g"]).reshape(256),
        "w_uq": f(inputs["w_uq"]).reshape(256, 768),
        "w_uk": f(inputs["w_uk"]).reshape(256, 512),
        "w_uv": f(inputs["w_uv"]).reshape(256, 512),
        "w_o": f(inputs["w_o"]).reshape(DM, DM),
        "ffn_norm_g": f(inputs["ffn_norm_g"]).reshape(DM),
        "peer_w_query": f(inputs["peer_w_query"]).reshape(DM, 2048),
        "peer_sub_keys": f(inputs["peer_sub_keys"]).reshape(16 * 128, 128),
        "peer_u": f(inputs["peer_u"]).reshape(16384, DM),
        "peer_v": f(inputs["peer_v"]).reshape(16384, DM),
        "final_norm_g": f(inputs["final_norm_g"]).reshape(DM),
    }
    shared.update(_CONSTS)
    return shared


def kernel(**inputs):
    shared = _layout_inputs(inputs)
    xs = np.asarray(inputs["x"], dtype=np.float32)
    n = xs.shape[0]
    nc = build_program()
    in_maps = []
    for b in range(n):
        m = dict(shared)
        m["x"] = np.ascontiguousarray(xs[b])
        in_maps.append(m)
    res = run_bass_kernel_spmd(nc, in_maps, core_ids=list(range(n)))
    return np.stack([np.asarray(r["out"], dtype=np.float32) for r in res.results], axis=0)
```
